# Optimizing a Trainium2 kernel written in Bass

```python
import jax, jax.numpy as jnp
from jax import lax
import numpy as np

D_MODEL = 1024
BATCH = 8
SEQ = 2048
DEPTH = 4

N_MEM = 256
RMS_EPS = 1e-6
N_SUBNORMS = 6

ML_HEADS = 4
ML_QK_DIM = D_MODEL // 2 // ML_HEADS
ML_V_DIM = D_MODEL // ML_HEADS
ML_CHUNK = 64
ML_GATE_CAP = 15.0
ML_IN_WIDTH = 2 * ML_HEADS * ML_QK_DIM + 2 * D_MODEL + 2 * ML_HEADS

SB_HEADS = 16
SB_HEAD_DIM = D_MODEL // SB_HEADS
SB_BLOCK = 128

XA_HEADS = 4
XA_HEAD_DIM = D_MODEL // XA_HEADS

D_FF = -(-8 * D_MODEL // (3 * 256)) * 256

N_ML_LAYERS = (DEPTH + 1) // 2
N_SB_LAYERS = DEPTH // 2

kernel_name = "hybrid_mlstm_stickbreak_memxattn_trunk"


def rms_norm(x, gain):
    x32 = x.astype(jnp.float32)
    y = x32 * lax.rsqrt(jnp.mean(x32 * x32, axis=-1, keepdims=True) + RMS_EPS)
    return (y * gain.astype(jnp.float32)).astype(x.dtype)


def mlstm_mixer(h, w_in, b_gate, head_gain, w_out):
    B, S, _ = h.shape
    H, dk, dv, L = ML_HEADS, ML_QK_DIM, ML_V_DIM, ML_CHUNK
    nc = S // L
    qk = H * dk
    proj = h @ w_in
    q, k, v, o, gates = jnp.split(proj, [qk, 2 * qk, 2 * qk + D_MODEL, 2 * qk + 2 * D_MODEL], axis=-1)
    gates = gates.astype(jnp.float32) + b_gate.astype(jnp.float32)
    gates = ML_GATE_CAP * jnp.tanh(gates / ML_GATE_CAP)
    ig = gates[..., :H]
    lf = jax.nn.log_sigmoid(gates[..., H:])

    def to_chunks(t, d):
        return t.astype(jnp.float32).reshape(B, nc, L, H, d).transpose(1, 0, 3, 2, 4)

    def gate_chunks(t):
        return t.reshape(B, nc, L, H).transpose(1, 0, 3, 2)

    qc = to_chunks(q, dk)
    kc = to_chunks(k, dk) * (dk ** -0.5)
    vc = to_chunks(v, dv)
    igc, lfc = gate_chunks(ig), gate_chunks(lf)
    causal = jnp.tril(jnp.ones((L, L), dtype=bool))

    def step(carry, inp):
        C, n, m = carry
        qb, kb, vb, igb, lfb = inp
        b = jnp.cumsum(lfb, axis=-1)
        log_d = jnp.where(causal, b[..., :, None] - b[..., None, :] + igb[..., None, :], -jnp.inf)
        m_inter = b + m[..., None]
        m_t = jnp.maximum(jnp.max(log_d, axis=-1), m_inter)
        d = jnp.exp(log_d - m_t[..., None])
        s = jnp.einsum('bhtd,bhsd->bhts', qb, kb) * d
        inter = jnp.exp(m_inter - m_t)
        num = jnp.einsum('bhts,bhsv->bhtv', s, vb) + inter[..., None] * jnp.einsum('bhvd,bhtd->bhtv', C, qb)
        den = jnp.sum(s, axis=-1) + inter * jnp.einsum('bhd,bhtd->bht', n, qb)
        h_out = num / jnp.maximum(jnp.abs(den), jnp.exp(-m_t))[..., None]
        b_last = b[..., -1]
        g = b_last[..., None] - b + igb
        m_new = jnp.maximum(b_last + m, jnp.max(g, axis=-1))
        w = jnp.exp(g - m_new[..., None])
        decay = jnp.exp(b_last + m - m_new)
        C = decay[..., None, None] * C + jnp.einsum('bhs,bhsv,bhsd->bhvd', w, vb, kb)
        n = decay[..., None] * n + jnp.einsum('bhs,bhsd->bhd', w, kb)
        return (C, n, m_new), h_out

    init = (jnp.zeros((B, H, dv, dk), jnp.float32),
            jnp.zeros((B, H, dk), jnp.float32),
            jnp.zeros((B, H), jnp.float32))
    _, hs = lax.scan(step, init, (qc, kc, vc, igc, lfc))
    hs = hs.transpose(1, 0, 3, 2, 4).reshape(B, S, H, dv)
    hs = rms_norm(hs, head_gain.reshape(H, dv))
    og = jax.nn.sigmoid(o.astype(jnp.float32)).reshape(B, S, H, dv)
    out = (hs * og).reshape(B, S, D_MODEL).astype(h.dtype)
    return out @ w_out


def stick_breaking_mixer(h, w_qkv, w_out):
    B, S, _ = h.shape
    H, dh = SB_HEADS, SB_HEAD_DIM
    qkv = (h @ w_qkv).reshape(B, S, 3, H, dh).astype(jnp.float32)
    q = qkv[:, :, 0].transpose(0, 2, 1, 3)
    k = qkv[:, :, 1].transpose(0, 2, 1, 3)
    v = qkv[:, :, 2].transpose(0, 2, 1, 3)
    scale = dh ** -0.5
    outs = []
    for blk in range(S // SB_BLOCK):
        q0 = blk * SB_BLOCK
        kend = q0 + SB_BLOCK
        qb = q[:, :, q0:kend]
        kb = k[:, :, :kend]
        vb = v[:, :, :kend]
        z = jnp.einsum('bhtd,bhsd->bhts', qb, kb) * scale
        t_idx = q0 + jnp.arange(SB_BLOCK)[:, None]
        s_idx = jnp.arange(kend)[None, :]
        strict = s_idx < t_idx
        log_1m = jnp.where(strict, jax.nn.log_sigmoid(-z), 0.0)
        suffix = lax.cumsum(log_1m, axis=3, reverse=True) - log_1m
        a = jnp.where(strict, jnp.exp(jax.nn.log_sigmoid(z) + suffix), 0.0)
        outs.append(jnp.einsum('bhts,bhsd->bhtd', a, vb))
    o = jnp.concatenate(outs, axis=2)
    o = o.transpose(0, 2, 1, 3).reshape(B, S, D_MODEL).astype(h.dtype)
    return o @ w_out


def memory_cross_attention(h, mem_n, w_q, w_kv, w_o):
    B, S, _ = h.shape
    q = (h @ w_q).reshape(B, S, XA_HEADS, XA_HEAD_DIM)
    kv = (mem_n @ w_kv).reshape(B, mem_n.shape[1], 2, XA_HEADS, XA_HEAD_DIM)
    k, v = kv[:, :, 0], kv[:, :, 1]
    s = jnp.einsum('bthd,bmhd->bhtm', q, k).astype(jnp.float32) * (XA_HEAD_DIM ** -0.5)
    p = jax.nn.softmax(s, axis=-1).astype(v.dtype)
    o = jnp.einsum('bhtm,bmhd->bthd', p, v).reshape(B, S, D_MODEL)
    return o @ w_o


def swiglu_ffn(h, w_gate_up, w_down):
    gate, up = jnp.split(h @ w_gate_up, 2, axis=-1)
    return (jax.nn.silu(gate) * up) @ w_down


def setup_inputs(seed: int = 0) -> dict:
    key = jax.random.key(seed)
    ks = jax.random.split(key, 16)

    def w(k, shape, fan_in):
        return jax.random.normal(k, shape, jnp.float32) * (fan_in ** -0.5)

    x = jax.random.normal(ks[0], (BATCH, SEQ, D_MODEL), jnp.float32)
    mem = jax.random.normal(ks[1], (BATCH, N_MEM, D_MODEL), jnp.float32)
    mem_norm_gain = 1.0 + 0.02 * jax.random.normal(ks[2], (D_MODEL,), jnp.float32)
    norm_gains = 1.0 + 0.02 * jax.random.normal(ks[3], (DEPTH, N_SUBNORMS, D_MODEL), jnp.float32)
    ml_w_in = w(ks[4], (N_ML_LAYERS, D_MODEL, ML_IN_WIDTH), D_MODEL)
    kb1, kb2 = jax.random.split(ks[5])
    b_in = 0.01 * jax.random.normal(kb1, (N_ML_LAYERS, ML_HEADS), jnp.float32)
    b_f = jnp.linspace(3.0, 6.0, ML_HEADS)[None, :] + 0.01 * jax.random.normal(kb2, (N_ML_LAYERS, ML_HEADS), jnp.float32)
    ml_b_gate = jnp.concatenate([b_in, b_f], axis=-1)
    ml_head_gain = 1.0 + 0.02 * jax.random.normal(ks[6], (N_ML_LAYERS, D_MODEL), jnp.float32)
    ml_w_out = w(ks[7], (N_ML_LAYERS, D_MODEL, D_MODEL), D_MODEL)
    sb_w_qkv = w(ks[8], (N_SB_LAYERS, D_MODEL, 3 * D_MODEL), D_MODEL)
    sb_w_out = w(ks[9], (N_SB_LAYERS, D_MODEL, D_MODEL), D_MODEL)
    xa_w_q = w(ks[10], (DEPTH, D_MODEL, D_MODEL), D_MODEL)
    xa_w_kv = w(ks[11], (DEPTH, D_MODEL, 2 * D_MODEL), D_MODEL)
    xa_w_o = w(ks[12], (DEPTH, D_MODEL, D_MODEL), D_MODEL)
    ffn_w_gate_up = w(ks[13], (DEPTH, D_MODEL, 2 * D_FF), D_MODEL)
    ffn_w_down = w(ks[14], (DEPTH, D_FF, D_MODEL), D_FF)
    return {"x": x, "mem": mem, "mem_norm_gain": mem_norm_gain, "norm_gains": norm_gains,
            "ml_w_in": ml_w_in, "ml_b_gate": ml_b_gate, "ml_head_gain": ml_head_gain, "ml_w_out": ml_w_out,
            "sb_w_qkv": sb_w_qkv, "sb_w_out": sb_w_out,
            "xa_w_q": xa_w_q, "xa_w_kv": xa_w_kv, "xa_w_o": xa_w_o,
            "ffn_w_gate_up": ffn_w_gate_up, "ffn_w_down": ffn_w_down}


def reference(x, mem, mem_norm_gain, norm_gains, ml_w_in, ml_b_gate, ml_head_gain, ml_w_out,
              sb_w_qkv, sb_w_out, xa_w_q, xa_w_kv, xa_w_o, ffn_w_gate_up, ffn_w_down):
    mem_n = rms_norm(mem, mem_norm_gain)
    for layer in range(DEPTH):
        g = norm_gains[layer]
        j = layer // 2
        hn = rms_norm(x, g[0])
        if layer % 2 == 0:
            y = mlstm_mixer(hn, ml_w_in[j], ml_b_gate[j], ml_head_gain[j], ml_w_out[j])
        else:
            y = stick_breaking_mixer(hn, sb_w_qkv[j], sb_w_out[j])
        x = x + rms_norm(y, g[1])
        y = memory_cross_attention(rms_norm(x, g[2]), mem_n, xa_w_q[layer], xa_w_kv[layer], xa_w_o[layer])
        x = x + rms_norm(y, g[3])
        y = swiglu_ffn(rms_norm(x, g[4]), ffn_w_gate_up[layer], ffn_w_down[layer])
        x = x + rms_norm(y, g[5])
    return x
```

```python
import numpy as np
from contextlib import ExitStack
import concourse.bass as bass
import concourse.mybir as mybir
from concourse.bass_utils import run_bass_kernel_spmd

F32 = mybir.dt.float32
BF16 = mybir.dt.bfloat16
AF = mybir.ActivationFunctionType
ALU = mybir.AluOpType
AX = mybir.AxisListType

D = 1024
S = 2048
NT = 16
NMEM = 256
F = 2816
NFC = 22
DEPTH = 4
EPS = 1e-6
NCORES = 8
MLW = 3080
PHASES = ("mix", "xa", "ffn")
SB_LA = 1


class Buf:
    __slots__ = ("name", "w", "r")

    def __init__(self, name=""):
        self.name = name
        self.w = None
        self.r = {}


class Eng:
    def __init__(self, name):
        self.name = name
        self.sem = None
        self.count = 0
        self.epoch = 0
        self.waited = {}
        self.q = []
        self.last = None

    @property
    def key(self):
        return "%s#%d" % (self.name, self.epoch)

    def wait(self, tok):
        if tok is None:
            return
        sem, val, key = tok
        if self.waited.get(key, 0) >= val:
            return
        self.waited[key] = val
        self.q.append(lambda e, sem=sem, val=val: e.wait_ge(sem, val))


class FW:
    NDMA = 16

    def __init__(self, nc, stack):
        self.nc = nc
        self.stack = stack
        self.engs = {}
        for name in ("pe", "act", "dve", "pool", "sp"):
            g = Eng(name)
            g.sem = stack.enter_context(nc.semaphore("sem_" + name))
            self.engs[name] = g
        self.pe, self.act, self.dve, self.pool, self.sp = (
            self.engs[k] for k in ("pe", "act", "dve", "pool", "sp"))
        self.dma_sems = []
        for i in range(self.NDMA):
            s = stack.enter_context(nc.semaphore("dsem%d" % i))
            self.dma_sems.append([s, 0, None])
        self.dma_i = 0
        self.nops = 0

    def _pre(self, eng, reads, writes):
        for b in reads:
            eng.wait(b.w)
        for b in writes:
            eng.wait(b.w)
            for t in b.r.values():
                eng.wait(t)

    def _post(self, tok, reads, writes):
        for b in reads:
            b.r[tok[2]] = tok
        for b in writes:
            b.w = tok
            b.r = {}

    def op(self, eng, fn, reads=(), writes=()):
        self._pre(eng, reads, writes)
        eng.count += 1
        tok = (eng.sem, eng.count, eng.key)
        eng.last = tok
        eng.q.append(lambda e, fn=fn, sem=eng.sem: fn(e).then_inc(sem, 1))
        self._post(tok, reads, writes)
        self.nops += 1
        return tok

    def dma(self, eng, out, in_, reads=(), writes=()):
        self._pre(eng, reads, writes)
        idx = self.dma_i % self.NDMA
        slot = self.dma_sems[idx]
        self.dma_i += 1
        if slot[2] is not None:
            eng.wait(slot[2])
        slot[1] += 16
        tok = (slot[0], slot[1], "dma%d" % idx)
        slot[2] = tok
        eng.q.append(lambda e, out=out, in_=in_, sem=slot[0]: e.dma_start(out=out, in_=in_).then_inc(sem, 16))
        self._post(tok, reads, writes)
        return tok

    def barrier(self):
        toks = [g.last for g in self.engs.values() if g.last is not None]
        toks += [s[2] for s in self.dma_sems if s[2] is not None]
        for g in self.engs.values():
            for t in toks:
                if t[2] != g.key:
                    g.wait(t)
        for g in self.engs.values():
            if g.count > 0:
                g.epoch += 1
                g.sem = self.stack.enter_context(self.nc.semaphore("sem_%s_%d" % (g.name, g.epoch)))
                g.count = 0
                g.last = None

    def emit(self):
        nc = self.nc
        with nc.Block() as block:
            @block.tensor
            def _(e):
                for f in self.pe.q:
                    f(e)

            @block.scalar
            def _(e):
                for f in self.act.q:
                    f(e)

            @block.vector
            def _(e):
                for f in self.dve.q:
                    f(e)

            @block.gpsimd
            def _(e):
                for f in self.pool.q:
                    f(e)

            @block.sync
            def _(e):
                for f in self.sp.q:
                    f(e)


class T:
    __slots__ = ("v", "b")

    def __init__(self, v, b=None, name=""):
        self.v = v
        self.b = b if b is not None else Buf(name)


class Rot:
    def __init__(self, items):
        self.items = items
        self.i = 0

    def next(self):
        it = self.items[self.i % len(self.items)]
        self.i += 1
        return it


class Arena:
    def __init__(self, ap, words):
        self.ap = ap
        self.words = words
        self.off = 0

    def alloc(self, shape, dt, name=""):
        assert shape[0] == 128
        n = 1
        for s in shape[1:]:
            n *= s
        nbytes = n * (2 if dt == BF16 else 4)
        w = (nbytes + 3) // 4
        w = (w + 7) // 8 * 8
        assert self.off + w <= self.words, ("arena overflow", name, self.off, w, self.words)
        v = self.ap[:, self.off:self.off + w]
        self.off += w
        if dt == BF16:
            v = v.bitcast(BF16)
        v = v[:, 0:n]
        if len(shape) == 3:
            v = v.rearrange("p (a b) -> p a b", a=shape[1])
        elif len(shape) == 4:
            v = v.rearrange("p (a b c) -> p a b c", a=shape[1], b=shape[2])
        return T(v, name=name)

    def rot(self, n, shape, dt, name=""):
        return Rot([self.alloc(shape, dt, name + str(i)) for i in range(n)])


def build_program(layer_ids):
    nc = bass.Bass("TRN2", target_bir_lowering=False)
    dr = lambda name, shape: nc.dram_tensor(name, shape, F32, kind="ExternalInput").ap()
    x_in = dr("x", [S, D])
    mem_in = dr("mem", [NMEM, D])
    gcol_in = dr("gcol", [128, 200])
    gbc_in = dr("gbc", [12, 128, D])
    hgbc_in = dr("hgbc", [2, 128, D])
    bgate_in = dr("bgate", [2, 128, 8])
    cstf_in = dr("constf", [128, 4, 128])
    cstb_in = dr("constb", [128, 6, 128])
    ml_w_in = dr("ml_w_in", [2, D, MLW])
    ml_w_out = dr("ml_w_out", [2, D, D])
    sb_w_qkv = dr("sb_w_qkv", [2, D, 3 * D])
    sb_w_out = dr("sb_w_out", [2, D, D])
    xa_w_q = dr("xa_w_q", [4, D, D])
    xa_w_kv = dr("xa_w_kv", [4, D, 2 * D])
    xa_w_o = dr("xa_w_o", [4, D, D])
    ffn_w_gu = dr("ffn_w_gate_up", [4, D, 2 * F])
    ffn_w_d = dr("ffn_w_down", [4, F, D])
    out_d = nc.dram_tensor("out", [S, D], F32, kind="ExternalOutput").ap()

    with ExitStack() as st:
        fw = FW(nc, st)
        pe, act, dve, pool, sp = fw.pe, fw.act, fw.dve, fw.pool, fw.sp
        sbt = lambda name, shape, dt: st.enter_context(nc.sbuf_tensor("sb_" + name, shape, dt))
        pst = lambda name, shape, dt: st.enter_context(nc.psum_tensor(name, shape, dt))

        X = sbt("X", [128, NT, D], F32)[:]
        XB = [Buf("X%d" % i) for i in range(NT)]
        cstf = T(sbt("cstf", [128, 4, 128], F32)[:], name="cstf")
        cstb = T(sbt("cstb", [128, 6, 128], BF16)[:], name="cstb")
        gcol = T(sbt("gcol", [128, 200], F32)[:], name="gcol")
        epsb = T(sbt("epsb", [128, 1], F32)[:], name="epsb")
        gbuf = T(sbt("gbuf", [128, D], F32)[:], name="gbuf")
        colt = sbt("colt", [128, 64], F32)[:]
        cols = Rot([T(colt[:, i:i + 1], name="col%d" % i) for i in range(64)])
        junk = T(sbt("junk", [128, D], BF16)[:], name="junk")
        xn_t = sbt("xn", [128, 2, D], BF16)[:]
        xnr = Rot([T(xn_t[:, i, :], name="xn%d" % i) for i in range(2)])
        tmp_t = sbt("tmpN", [128, 2, 512], F32)[:]
        tmpN = Rot([T(tmp_t[:, i, :], name="tmpN%d" % i) for i in range(2)])
        AW = (nc.sbuf_bytes_remaining - 64) // 4 // 8 * 8
        arena_t = sbt("arena", [128, AW], F32)[:]
        arena = Arena(arena_t, AW)
        pds = [pst("pd%d" % i, [128, 2, 512], F32)[:] for i in range(4)]
        banks = [T(pds[i // 2][:, i % 2, :], name="ps%d" % i) for i in range(8)]
        psM = Rot(banks[0:4])
        psY = Rot(banks[4:6])
        psT = Rot(banks[6:8])
        psY6 = Rot(banks[0:6])

        identf = cstf.v[:, 0, :]
        trif = cstf.v[:, 1, :]
        negmf = cstf.v[:, 2, :]
        onesf = cstf.v[:, 3, :]
        identb = cstb.v[:, 0, :]
        usufb = cstb.v[:, 1, :]
        nonesb = cstb.v[:, 2, :]
        negsb = cstb.v[:, 3, :]
        mstrictb = cstb.v[:, 4, :]
        zerosb = cstb.v[:, 5, :]

        def mm(out_ap, pairs, reads, writes):
            def f(e, out_ap=out_ap, pairs=pairs):
                n = len(pairs)
                ins = None
                for i, (l, r) in enumerate(pairs):
                    ins = e.matmul(out_ap, lhsT=l, rhs=r, start=(i == 0), stop=(i == n - 1))
                return ins
            return fw.op(pe, f, reads, writes)

        def bf16_bank(bank):
            return bank.v.bitcast(BF16).rearrange("p (c f) -> p c f", c=8)

        def transposes(bank, srcs, reads):
            pv = bf16_bank(bank)

            def f(e, pv=pv, srcs=srcs):
                ins = None
                for i, s_ in enumerate(srcs):
                    ins = e.transpose(out=pv[:, i, :], in_=s_, identity=identb)
                return ins
            fw.op(pe, f, list(reads) + [cstb.b], [bank.b])
            return pv

        def rstd_from(ssum, scale):
            r = cols.next()
            fw.op(act, lambda e: e.activation(out=r.v, in_=ssum.v, func=AF.Ln, scale=scale, bias=epsb.v[:, 0:1]),
                  [ssum.b, epsb.b], [r.b])
            fw.op(act, lambda e: e.activation(out=r.v, in_=r.v, func=AF.Exp, scale=-0.5), [], [r.b])
            return r

        def norm_T(srcs, gi, hT, evac_alt=0):
            for k, (sv, sbuf_) in enumerate(srcs):
                s_ = cols.next()
                fw.op(act, lambda e, sv=sv, s_=s_: e.activation(out=junk.v, in_=sv, func=AF.Square, accum_out=s_.v),
                      [sbuf_], [junk.b, s_.b])
                r = rstd_from(s_, 1.0 / D)
                xn = xnr.next()
                fw.op(dve, lambda e, sv=sv, xn=xn, r=r: e.tensor_scalar(out=xn.v, in0=sv, scalar1=r.v, scalar2=None,
                                                                        op0=ALU.mult),
                      [sbuf_, r.b], [xn.b])
                bank = psT.next()
                pv = transposes(bank, [xn.v[:, c * 128:(c + 1) * 128] for c in range(8)], [xn.b])
                g3 = gcol.v[:, gi * 8:(gi + 1) * 8].rearrange("p (c o) -> p c o", o=1).to_broadcast([128, 8, 128])
                fw.op(dve, lambda e, pv=pv, k=k, g3=g3: e.tensor_tensor(out=hT.v[:, :, k * 128:(k + 1) * 128], in0=pv,
                                                                       in1=g3, op=ALU.mult),
                      [bank.b, gcol.b], [hT.b])

        def post_norm(tt, y0, y1):
            s0 = cols.next()
            s1 = cols.next()
            fw.op(act, lambda e: e.activation(out=junk.v[:, 0:512], in_=y0.v[:, :], func=AF.Square, accum_out=s0.v),
                  [y0.b], [junk.b, s0.b])
            fw.op(act, lambda e: e.activation(out=junk.v[:, 512:1024], in_=y1.v[:, :], func=AF.Square, accum_out=s1.v),
                  [y1.b], [junk.b, s1.b])
            ssum = cols.next()
            fw.op(dve, lambda e: e.tensor_tensor(out=ssum.v, in0=s0.v, in1=s1.v, op=ALU.add), [s0.b, s1.b], [ssum.b])
            r = rstd_from(ssum, 1.0 / D)
            for h, y in enumerate((y0, y1)):
                t = tmpN.next()
                fw.op(dve, lambda e, y=y, t=t, h=h: e.scalar_tensor_tensor(
                    out=t.v, in0=y.v[:, :], scalar=r.v, in1=gbuf.v[:, h * 512:(h + 1) * 512], op0=ALU.mult, op1=ALU.mult),
                    [y.b, r.b, gbuf.b], [t.b])
                fw.op(pool, lambda e, t=t, h=h: e.tensor_tensor(
                    out=X[:, tt, h * 512:(h + 1) * 512], in0=X[:, tt, h * 512:(h + 1) * 512], in1=t.v, op=ALU.add),
                    [t.b], [XB[tt]])

        def load_w(dst, src2d, nchunk, f0, f1, step=1024):
            srcv = src2d.rearrange("(c p) f -> p c f", p=128)
            for a in range(f0, f1, step):
                b = min(a + step, f1)
                fw.dma(pool, dst.v[:, :, a - f0:b - f0], srcv[:, :, a:b], [], [dst.b])

        def out_proj(inT, kslice, w, nk, tt, psY=psY):
            ys = []
            for half in range(2):
                y = psY.next()
                mm(y.v[:, :], [(inT.v[:, c, kslice], w.v[:, c, half * 512:(half + 1) * 512]) for c in range(nk)],
                   [inT.b, w.b], [y.b])
                ys.append(y)
            post_norm(tt, ys[0], ys[1])

        evac_ctr = [0]

        def evac(out_ap, in_ap, reads, writes, scale=None, force=None):
            evac_ctr[0] += 1
            if (evac_ctr[0] % 2 == 0 and force is None) or force == "act":
                if scale is None:
                    fw.op(act, lambda e: e.copy(out=out_ap, in_=in_ap), reads, writes)
                else:
                    fw.op(act, lambda e: e.mul(out=out_ap, in_=in_ap, mul=scale), reads, writes)
            else:
                if scale is None:
                    fw.op(dve, lambda e: e.tensor_copy(out=out_ap, in_=in_ap), reads, writes)
                else:
                    fw.op(dve, lambda e: e.tensor_scalar(out=out_ap, in0=in_ap, scalar1=scale, scalar2=None,
                                                         op0=ALU.mult), reads, writes)

        def load_gain(idx):
            fw.dma(sp, gbuf.v, gbc_in[idx], [], [gbuf.b])

        fw.dma(sp, X[:, :, :], x_in.rearrange("(t p) f -> p t f", p=128), [], XB)
        fw.dma(sp, cstf.v, cstf_in, [], [cstf.b])
        fw.dma(pool, cstb.v, cstb_in, [], [cstb.b])
        fw.dma(sp, gcol.v, gcol_in, [], [gcol.b])
        fw.op(pool, lambda e: e.memset(epsb.v, EPS), [], [epsb.b])

        def xattn_phase(L):
            fw.barrier()
            arena.off = 0
            wq = arena.alloc([128, 8, D], BF16, "wq")
            wo = arena.alloc([128, 8, D], BF16, "wo")
            kmT = arena.alloc([128, 8, NMEM], BF16, "kmT")
            vm = arena.alloc([128, 2, D], BF16, "vm")
            mark = arena.off
            wkv = arena.alloc([128, 8, 2 * D], BF16, "wkv")
            memt = arena.alloc([128, 2, D], F32, "memt")
            memnT = arena.alloc([128, 8, NMEM], BF16, "memnT")
            load_gain(L * 3 + 1)
            fw.dma(sp, memt.v, mem_in.rearrange("(t p) f -> p t f", p=128), [], [memt.b])
            load_w(wkv, xa_w_kv[L], 8, 0, 2 * D)
            load_w(wq, xa_w_q[L], 8, 0, D)
            load_w(wo, xa_w_o[L], 8, 0, D)
            norm_T([(memt.v[:, i, :], memt.b) for i in range(2)], 24, memnT)
            for jc in range(8):
                bk = psM.next()
                mm(bk.v[:, 0:NMEM], [(wkv.v[:, c, jc * 128:(jc + 1) * 128], memnT.v[:, c, :]) for c in range(8)],
                   [wkv.b, memnT.b], [bk.b])
                evac(kmT.v[:, jc, :], bk.v[:, 0:NMEM], [bk.b], [kmT.b])
            for mt in range(2):
                for half in range(2):
                    bk = psM.next()
                    mm(bk.v[:, :], [(memnT.v[:, c, mt * 128:(mt + 1) * 128],
                                     wkv.v[:, c, D + half * 512:D + (half + 1) * 512]) for c in range(8)],
                       [wkv.b, memnT.b], [bk.b])
                    evac(vm.v[:, mt, half * 512:(half + 1) * 512], bk.v[:, :], [bk.b], [vm.b])
            fw.barrier()
            arena.off = mark
            hTr = arena.rot(2, [128, 8, 512], BF16, "hT")
            qTr = arena.rot(2, [128, 8, 512], BF16, "qT")
            Pr = arena.rot(3, [128, 4, NMEM], BF16, "P")
            PTr = arena.rot(2, [128, 8, 128], BF16, "PT")
            otr = arena.rot(3, [128, D], BF16, "otok")
            oTr = arena.rot(2, [128, 8, 128], BF16, "oT")
            st4 = arena.rot(4, [128, 16], F32, "st4")
            sc = 1.0 / 16.0
            ctx = {}

            def prep(g):
                hT = hTr.next()
                qT = qTr.next()
                norm_T([(X[:, 4 * g + k, :], XB[4 * g + k]) for k in range(4)], L * 6 + 2, hT)
                for jc in range(8):
                    bk = psM.next()
                    mm(bk.v[:, :], [(wq.v[:, c, jc * 128:(jc + 1) * 128], hT.v[:, c, :]) for c in range(8)],
                       [wq.b, hT.b], [bk.b])
                    evac(qT.v[:, jc, :], bk.v[:, :], [bk.b], [qT.b])
                ctx["qT%d" % g] = qT

            def s0(tt):
                g, k = tt // 4, tt % 4
                if tt == 0:
                    prep(0)
                if k == 2 and g + 1 < 4:
                    prep(g + 1)
                qT = ctx["qT%d" % g]
                ks = slice(k * 128, (k + 1) * 128)
                P = Pr.next()
                s4 = st4.next()
                for hp in range(2):
                    bk = psM.next()

                    def fsc(e, bk=bk, hp=hp, ks=ks, qT=qT):
                        ins = None
                        for hh in range(2):
                            h = 2 * hp + hh
                            for dc in range(2):
                                ins = e.matmul(bk.v[:, hh * NMEM:(hh + 1) * NMEM], lhsT=qT.v[:, 2 * h + dc, ks],
                                               rhs=kmT.v[:, 2 * h + dc, :], start=(dc == 0), stop=(dc == 1))
                        return ins
                    fw.op(pe, fsc, [qT.b, kmT.b], [bk.b])
                    fw.op(dve, lambda e, bk=bk, hp=hp, s4=s4: e.tensor_reduce(
                        out=s4.v[:, 2 * hp:2 * hp + 2], in_=bk.v[:, :].rearrange("p (h m) -> p h m", h=2),
                        axis=AX.X, op=ALU.max), [bk.b], [s4.b])
                    fw.op(dve, lambda e, hp=hp, s4=s4: e.tensor_scalar(
                        out=s4.v[:, 2 * hp:2 * hp + 2], in0=s4.v[:, 2 * hp:2 * hp + 2], scalar1=-sc, scalar2=None,
                        op0=ALU.mult), [], [s4.b])
                    for hh in range(2):
                        h = 2 * hp + hh
                        fw.op(act, lambda e, bk=bk, hh=hh, h=h, P=P, s4=s4: e.activation(
                            out=P.v[:, h, :], in_=bk.v[:, hh * NMEM:(hh + 1) * NMEM], func=AF.Exp, scale=sc,
                            bias=s4.v[:, h:h + 1], accum_out=s4.v[:, 4 + h:5 + h]), [bk.b, s4.b], [P.b, s4.b])
                fw.op(act, lambda e, s4=s4: e.activation(out=s4.v[:, 8:12], in_=s4.v[:, 4:8], func=AF.Ln),
                      [s4.b], [s4.b])
                fw.op(act, lambda e, s4=s4: e.activation(out=s4.v[:, 8:12], in_=s4.v[:, 8:12], func=AF.Exp,
                                                        scale=-1.0), [], [s4.b])
                ctx["P%d" % tt] = (P, s4)

            def s1(tt):
                P, s4 = ctx.pop("P%d" % tt)
                bkT = psT.next()
                pv = transposes(bkT, [P.v[:, h, mt * 128:(mt + 1) * 128] for h in range(4) for mt in range(2)],
                                [P.b])
                PT = PTr.next()
                evac(PT.v, pv, [bkT.b], [PT.b])
                ot = otr.next()
                for hp in range(2):
                    bk = psM.next()

                    def fpv(e, bk=bk, hp=hp, PT=PT):
                        ins = None
                        for hh in range(2):
                            h = 2 * hp + hh
                            for mt in range(2):
                                ins = e.matmul(bk.v[:, hh * 256:(hh + 1) * 256], lhsT=PT.v[:, 2 * h + mt, :],
                                               rhs=vm.v[:, mt, h * 256:(h + 1) * 256], start=(mt == 0),
                                               stop=(mt == 1))
                        return ins
                    fw.op(pe, fpv, [PT.b, vm.b], [bk.b])
                    rb = s4.v[:, 8 + 2 * hp:10 + 2 * hp].rearrange("p (h o) -> p h o", o=1).to_broadcast(
                        [128, 2, 256])
                    fw.op(dve, lambda e, bk=bk, hp=hp, ot=ot, rb=rb: e.tensor_tensor(
                        out=ot.v[:, hp * 512:(hp + 1) * 512].rearrange("p (h v) -> p h v", h=2),
                        in0=bk.v[:, :].rearrange("p (h v) -> p h v", h=2), in1=rb, op=ALU.mult),
                        [bk.b, s4.b], [ot.b])
                ctx["ot%d" % tt] = ot

            def s2(tt):
                ot = ctx.pop("ot%d" % tt)
                bkT = psT.next()
                pv = transposes(bkT, [ot.v[:, c * 128:(c + 1) * 128] for c in range(8)], [ot.b])
                oT = oTr.next()
                evac(oT.v, pv, [bkT.b], [oT.b])
                out_proj(oT, slice(0, 128), wo, 8, tt)

            stages = [(s0, 0), (s1, 2), (s2, 4)]
            for step in range(NT + 4):
                for fn_, off in reversed(stages):
                    tt = step - off
                    if 0 <= tt < NT:
                        fn_(tt)

        def ffn_phase(L):
            fw.barrier()
            arena.off = 0
            wd = arena.alloc([128, NFC, D], BF16, "wd")
            actT = arena.alloc([128, NFC, 1024], BF16, "actT")
            hT = arena.alloc([128, 8, 1024], BF16, "hT")
            pcs = arena.rot(2, [128, 8, 2, 256], BF16, "pc")
            stmp = tmpN
            load_gain(L * 3 + 2)
            wdv = ffn_w_d[L].rearrange("(j p) f -> p j f", p=128)
            for a in range(0, NFC, 6):
                b = min(a + 6, NFC)
                fw.dma(pool, wd.v[:, a:b, :], wdv[:, a:b, :], [], [wd.b])
            wguv = ffn_w_gu[L].rearrange("(c p) f -> p c f", p=128)
            norm_T([(X[:, k, :], XB[k]) for k in range(8)], L * 6 + 4, hT)
            for G in range(2):
                for jp in range(NFC // 2):
                    pc = pcs.next()
                    fw.dma(pool, pc.v[:, :, 0, :], wguv[:, :, jp * 256:(jp + 1) * 256], [], [pc.b])
                    fw.dma(pool, pc.v[:, :, 1, :], wguv[:, :, F + jp * 256:F + (jp + 1) * 256], [], [pc.b])
                    for jj in range(2):
                        j = 2 * jp + jj
                        for hf in range(2):
                            ts = slice(hf * 512, (hf + 1) * 512)
                            bg_ = psM.next()
                            mm(bg_.v[:, :], [(pc.v[:, c, 0, jj * 128:(jj + 1) * 128], hT.v[:, c, ts]) for c in range(8)],
                               [pc.b, hT.b], [bg_.b])
                            bu_ = psM.next()
                            mm(bu_.v[:, :], [(pc.v[:, c, 1, jj * 128:(jj + 1) * 128], hT.v[:, c, ts]) for c in range(8)],
                               [pc.b, hT.b], [bu_.b])
                            sg = stmp.next()
                            fw.op(act, lambda e, bg_=bg_, sg=sg: e.activation(out=sg.v, in_=bg_.v[:, :], func=AF.Silu),
                                  [bg_.b], [sg.b])
                            fw.op(dve, lambda e, bu_=bu_, sg=sg, j=j, ts=ts: e.tensor_tensor(
                                out=actT.v[:, j, ts], in0=bu_.v[:, :], in1=sg.v, op=ALU.mult), [bu_.b, sg.b], [actT.b])
                if G == 0:
                    norm_T([(X[:, 8 + k, :], XB[8 + k]) for k in range(8)], L * 6 + 4, hT)
                for k in range(8):
                    out_proj(actT, slice(k * 128, (k + 1) * 128), wd, NFC, 8 * G + k, psY=psY6)

        def mlstm_phase(L, j):
            fw.barrier()
            arena.off = 0
            win = arena.alloc([128, 8, MLW], BF16, "win")
            wout = arena.alloc([128, 8, D], BF16, "wout")
            hg = arena.alloc([128, D], F32, "hg")
            bgt = arena.alloc([128, 8], F32, "bg")
            hT = arena.alloc([128, 8, 512], BF16, "hT")
            qtr = arena.rot(2, [128, 512], BF16, "qtok")
            ktr = arena.rot(2, [128, 512], BF16, "ktok")
            var = arena.rot(2, [128, 4, 264], BF16, "vaug")
            ogr = arena.rot(2, [128, D], F32, "og")
            qkTr = arena.rot(2, [128, 8, 128], BF16, "qkT")
            Kwr = arena.rot(2, [128, 4, 128], BF16, "Kw")
            g8r = arena.rot(2, [128, 40], F32, "g8")
            Dtr = arena.rot(2, [128, 128], F32, "Dt")
            Str = arena.rot(2, [128, 128], BF16, "St")
            isbr = arena.rot(2, [128, 264], F32, "isb")
            Hsr = arena.rot(2, [128, 264], F32, "Hs")
            C32 = [arena.alloc([128, 264], F32, "C32_%d" % h) for h in range(4)]
            Cb = [arena.alloc([128, 264], BF16, "Cb_%d" % h) for h in range(4)]
            mor = arena.rot(2, [128, D], BF16, "mo")
            moTr = arena.rot(2, [128, 8, 128], BF16, "moT")
            load_gain(L * 3 + 0)
            load_w(win, ml_w_in[j], 8, 0, MLW)
            load_w(wout, ml_w_out[j], 8, 0, D)
            fw.dma(sp, hg.v, hgbc_in[j], [], [hg.b])
            fw.dma(sp, bgt.v, bgate_in[j], [], [bgt.b])
            for h in range(4):
                fw.op(pool, lambda e, h=h: e.memset(C32[h].v, 0.0), [], [C32[h].b])
                fw.op(pool, lambda e, h=h: e.memset(Cb[h].v, 0.0), [], [Cb[h].b])
            for va in var.items:
                fw.op(pool, lambda e, va=va: e.memset(va.v, 1.0), [], [va.b])
            KSC = 128.0 ** -0.5
            mctx = {}

            def ms0(tt):
                g, k = tt // 4, tt % 4
                if k == 0:
                    norm_T([(X[:, 4 * g + kk, :], XB[4 * g + kk]) for kk in range(4)], L * 6 + 0, hT)
                ks = slice(k * 128, (k + 1) * 128)

                def proj(c0, c1):
                    bk = psM.next()
                    mm(bk.v[:, 0:c1 - c0], [(hT.v[:, c, ks], win.v[:, c, c0:c1]) for c in range(8)],
                       [hT.b, win.b], [bk.b])
                    return bk
                qt = qtr.next()
                kt = ktr.next()
                va = var.next()
                og = ogr.next()
                g8 = g8r.next()
                bk = proj(0, 512)
                fw.op(act, lambda e, bk=bk, qt=qt: e.copy(out=qt.v, in_=bk.v[:, :]), [bk.b], [qt.b])
                bk = proj(512, 1024)
                fw.op(dve, lambda e, bk=bk, kt=kt: e.tensor_scalar(out=kt.v, in0=bk.v[:, :], scalar1=KSC,
                                                                   scalar2=None, op0=ALU.mult), [bk.b], [kt.b])
                for i in range(2):
                    bk = proj(1024 + i * 512, 1536 + i * 512)
                    evac(va.v[:, 2 * i:2 * i + 2, 0:256], bk.v[:, :].rearrange("p (h v) -> p h v", h=2),
                         [bk.b], [va.b])
                for i in range(2):
                    bk = proj(2048 + i * 512, 2560 + i * 512)
                    fw.op(act, lambda e, bk=bk, og=og, i=i: e.activation(out=og.v[:, i * 512:(i + 1) * 512],
                                                                        in_=bk.v[:, :], func=AF.Sigmoid),
                          [bk.b], [og.b])
                fw.op(pool, lambda e, og=og: e.tensor_tensor(out=og.v, in0=og.v, in1=hg.v, op=ALU.mult),
                      [hg.b], [og.b])
                bk = proj(3072, 3080)
                fw.op(dve, lambda e, bk=bk, g8=g8: e.tensor_tensor(out=g8.v[:, 0:8], in0=bk.v[:, 0:8], in1=bgt.v,
                                                                  op=ALU.add), [bk.b, bgt.b], [g8.b])
                fw.op(act, lambda e, g8=g8: e.activation(out=g8.v[:, 0:8], in_=g8.v[:, 0:8], func=AF.Tanh,
                                                        scale=1.0 / 15.0), [], [g8.b])
                fw.op(dve, lambda e, g8=g8: e.tensor_scalar(out=g8.v[:, 0:8], in0=g8.v[:, 0:8], scalar1=15.0,
                                                           scalar2=None, op0=ALU.mult), [], [g8.b])
                fw.op(act, lambda e, g8=g8: e.activation(out=g8.v[:, 8:12], in_=g8.v[:, 4:8], func=AF.Exp,
                                                        scale=-1.0), [], [g8.b])
                fw.op(act, lambda e, g8=g8: e.activation(out=g8.v[:, 8:12], in_=g8.v[:, 8:12], func=AF.Ln,
                                                        bias=1.0), [], [g8.b])
                fw.op(dve, lambda e, g8=g8: e.tensor_scalar(out=g8.v[:, 8:12], in0=g8.v[:, 8:12], scalar1=-1.0,
                                                           scalar2=None, op0=ALU.mult), [], [g8.b])
                bkc = psT.next()

                def fcs(e, bkc=bkc, g8=g8):
                    e.matmul(bkc.v[:, 0:4], lhsT=trif, rhs=g8.v[:, 8:12], start=True, stop=True)
                    return e.matmul(bkc.v[:, 4:8], lhsT=onesf, rhs=g8.v[:, 8:12], start=True, stop=True)
                fw.op(pe, fcs, [g8.b, cstf.b], [bkc.b])
                fw.op(dve, lambda e, bkc=bkc, g8=g8: e.tensor_copy(out=g8.v[:, 28:36], in_=bkc.v[:, 0:8]),
                      [bkc.b], [g8.b])
                fw.op(dve, lambda e, g8=g8: e.tensor_tensor(out=g8.v[:, 12:16], in0=g8.v[:, 0:4],
                                                           in1=g8.v[:, 28:32], op=ALU.subtract), [], [g8.b])
                fw.op(dve, lambda e, g8=g8: e.tensor_tensor(out=g8.v[:, 20:24], in0=g8.v[:, 12:16],
                                                           in1=g8.v[:, 32:36], op=ALU.add), [], [g8.b])
                fw.op(dve, lambda e, g8=g8: e.tensor_copy(out=g8.v[:, 24:28], in_=g8.v[:, 32:36]), [], [g8.b])
                fw.op(act, lambda e, g8=g8: e.activation(out=g8.v[:, 16:20], in_=g8.v[:, 28:32], func=AF.Exp),
                      [], [g8.b])
                fw.op(act, lambda e, g8=g8: e.activation(out=g8.v[:, 20:28], in_=g8.v[:, 20:28], func=AF.Exp),
                      [], [g8.b])
                bkT = psT.next()
                pv = transposes(bkT, [qt.v[:, h * 128:(h + 1) * 128] for h in range(4)] +
                                [kt.v[:, h * 128:(h + 1) * 128] for h in range(4)], [qt.b, kt.b])
                qkT = qkTr.next()
                evac(qkT.v, pv, [bkT.b], [qkT.b])
                Kw = Kwr.next()
                wb = g8.v[:, 20:24].rearrange("p (h o) -> p h o", o=1).to_broadcast([128, 4, 128])
                fw.op(dve, lambda e, Kw=Kw, kt=kt, wb=wb: e.tensor_tensor(
                    out=Kw.v, in0=kt.v.rearrange("p (h d) -> p h d", h=4), in1=wb, op=ALU.mult),
                    [kt.b, g8.b], [Kw.b])
                mctx["a%d" % tt] = (va, og, g8, qkT, Kw)

            def ms1(tt):
                va, og, g8, qkT, Kw = mctx.pop("a%d" % tt)
                mo = mor.next()
                def hgen(h):
                    bkA = psM.next()

                    def fA(e, bkA=bkA, g8=g8, qkT=qkT, h=h):
                        e.matmul(bkA.v[:, 0:128], lhsT=g8.v[:, 8 + h:9 + h].to_broadcast([128, 128]), rhs=trif,
                                 start=True, stop=False)
                        e.matmul(bkA.v[:, 0:128], lhsT=identf, rhs=negmf, start=False, stop=True)
                        return e.matmul(bkA.v[:, 128:256], lhsT=qkT.v[:, 4 + h, :], rhs=qkT.v[:, h, :],
                                        start=True, stop=True)
                    fw.op(pe, fA, [g8.b, qkT.b, cstf.b], [bkA.b])
                    yield
                    Dt = Dtr.next()
                    fw.op(act, lambda e, bkA=bkA, Dt=Dt, g8=g8, h=h: e.activation(
                        out=Dt.v, in_=bkA.v[:, 0:128], func=AF.Exp, bias=g8.v[:, 12 + h:13 + h]),
                        [bkA.b, g8.b], [Dt.b])
                    yield
                    St = Str.next()
                    fw.op(dve, lambda e, bkA=bkA, Dt=Dt, St=St: e.tensor_tensor(
                        out=St.v, in0=bkA.v[:, 128:256], in1=Dt.v, op=ALU.mult), [bkA.b, Dt.b], [St.b])
                    yield
                    bkI = psM.next()
                    mm(bkI.v[:, 0:257], [(St.v, va.v[:, h, 0:257])], [St.b, va.b], [bkI.b])
                    yield
                    bkN = psM.next()
                    mm(bkN.v[:, 0:257], [(qkT.v[:, h, :], Cb[h].v[:, 0:257])], [qkT.b, Cb[h].b], [bkN.b])
                    yield
                    isb = isbr.next()
                    fw.op(act, lambda e, bkN=bkN, isb=isb, g8=g8, h=h: e.activation(
                        out=isb.v[:, 0:257], in_=bkN.v[:, 0:257], func=AF.Copy, scale=g8.v[:, 16 + h:17 + h]),
                        [bkN.b, g8.b], [isb.b])
                    yield
                    Hs = Hsr.next()
                    fw.op(dve, lambda e, bkI=bkI, isb=isb, Hs=Hs: e.tensor_tensor(
                        out=Hs.v[:, 0:257], in0=bkI.v[:, 0:257], in1=isb.v[:, 0:257], op=ALU.add),
                        [bkI.b, isb.b], [Hs.b])
                    yield
                    c1 = cols.next()
                    fw.op(dve, lambda e, Hs=Hs, c1=c1: e.tensor_scalar(
                        out=c1.v, in0=Hs.v[:, 256:257], scalar1=-1.0, scalar2=None, op0=ALU.mult),
                        [Hs.b], [c1.b])
                    yield
                    fw.op(dve, lambda e, Hs=Hs, c1=c1: e.tensor_tensor(
                        out=c1.v, in0=c1.v, in1=Hs.v[:, 256:257], op=ALU.max), [Hs.b], [c1.b])
                    yield
                    fw.op(dve, lambda e, c1=c1: e.tensor_scalar(
                        out=c1.v, in0=c1.v, scalar1=1.0, scalar2=None, op0=ALU.max), [], [c1.b])
                    yield
                    fw.op(act, lambda e, c1=c1: e.activation(out=c1.v, in_=c1.v, func=AF.Ln), [c1.b], [c1.b])
                    yield
                    fw.op(act, lambda e, c1=c1: e.activation(out=c1.v, in_=c1.v, func=AF.Exp, scale=-1.0),
                          [], [c1.b])
                    yield
                    ssh = cols.next()
                    fw.op(act, lambda e, Hs=Hs, c1=c1, ssh=ssh: e.activation(
                        out=junk.v[:, 0:256], in_=Hs.v[:, 0:256], func=AF.Square, scale=c1.v, accum_out=ssh.v),
                        [Hs.b, c1.b], [junk.b, ssh.b])
                    yield
                    r = rstd_from(ssh, 1.0 / 256.0)
                    yield
                    fw.op(dve, lambda e, r=r, c1=c1: e.tensor_tensor(out=r.v, in0=r.v, in1=c1.v, op=ALU.mult),
                          [c1.b], [r.b])
                    yield
                    fw.op(dve, lambda e, Hs=Hs, r=r, og=og, mo=mo, h=h: e.scalar_tensor_tensor(
                        out=mo.v[:, h * 256:(h + 1) * 256], in0=Hs.v[:, 0:256], scalar=r.v,
                        in1=og.v[:, h * 256:(h + 1) * 256], op0=ALU.mult, op1=ALU.mult),
                        [Hs.b, r.b, og.b], [mo.b])
                    yield
                    bkU = psM.next()
                    mm(bkU.v[:, 0:257], [(Kw.v[:, h, :], va.v[:, h, 0:257])], [Kw.b, va.b], [bkU.b])
                    yield
                    fw.op(dve, lambda e, bkU=bkU, g8=g8, h=h: e.scalar_tensor_tensor(
                        out=C32[h].v[:, 0:257], in0=C32[h].v[:, 0:257], scalar=g8.v[:, 24 + h:25 + h],
                        in1=bkU.v[:, 0:257], op0=ALU.mult, op1=ALU.add), [bkU.b, g8.b], [C32[h].b])
                    yield
                    fw.op(pool, lambda e, h=h: e.tensor_copy(out=Cb[h].v[:, 0:257], in_=C32[h].v[:, 0:257]),
                          [C32[h].b], [Cb[h].b])
                    yield

                for pair in ((0, 1), (2, 3)):
                    gens = [hgen(h) for h in pair]
                    while gens:
                        for g_ in list(gens):
                            try:
                                next(g_)
                            except StopIteration:
                                gens.remove(g_)
                mctx["m%d" % tt] = mo

            def ms2(tt):
                mo = mctx.pop("m%d" % tt)
                bkT = psT.next()
                pv = transposes(bkT, [mo.v[:, c * 128:(c + 1) * 128] for c in range(8)], [mo.b])
                moT = moTr.next()
                evac(moT.v, pv, [bkT.b], [moT.b])
                out_proj(moT, slice(0, 128), wout, 8, tt)

            mstages = [ms0, ms1, ms2]
            for step in range(NT + len(mstages) - 1):
                for si in reversed(range(len(mstages))):
                    tt = step - si
                    if 0 <= tt < NT:
                        mstages[si](tt)

        def sb_phase(L, j):
            fw.barrier()
            arena.off = 0
            KT = arena.alloc([128, 8, S], BF16, "KT")
            V = arena.alloc([128, NT, D], BF16, "V")
            mark = arena.off
            wkv = arena.alloc([128, 8, 2 * D], BF16, "wkv")
            hTs = [arena.alloc([128, 8, 512], BF16, "hTa"), arena.alloc([128, 8, 512], BF16, "hTb")]
            load_gain(L * 3 + 0)
            load_w(wkv, sb_w_qkv[j], 8, D, 3 * D)
            norm_T([(X[:, k, :], XB[k]) for k in range(4)], L * 6 + 0, hTs[0])
            for g in range(4):
                hT = hTs[g % 2]
                if g + 1 < 4:
                    norm_T([(X[:, 4 * (g + 1) + k, :], XB[4 * (g + 1) + k]) for k in range(4)], L * 6 + 0,
                           hTs[(g + 1) % 2])
                for jc in range(8):
                    bk = psM.next()
                    mm(bk.v[:, :], [(wkv.v[:, c, jc * 128:(jc + 1) * 128], hT.v[:, c, :]) for c in range(8)],
                       [wkv.b, hT.b], [bk.b])
                    evac(KT.v[:, jc, g * 512:(g + 1) * 512], bk.v[:, :], [bk.b], [KT.b], scale=0.125)
                for k in range(4):
                    for half in range(2):
                        bk = psM.next()
                        mm(bk.v[:, :], [(hT.v[:, c, k * 128:(k + 1) * 128],
                                         wkv.v[:, c, D + half * 512:D + (half + 1) * 512]) for c in range(8)],
                           [wkv.b, hT.b], [bk.b])
                        evac(V.v[:, 4 * g + k, half * 512:(half + 1) * 512], bk.v[:, :], [bk.b], [V.b])
            fw.barrier()
            arena.off = mark
            wq = arena.alloc([128, 8, D], BF16, "wq")
            wout = arena.alloc([128, 8, D], BF16, "wout")
            hT = arena.alloc([128, 8, 512], BF16, "hT2")
            oT = hT
            qT = arena.alloc([128, 8, 512], BF16, "qT")
            spr = arena.rot(2, [128, 2, 512], BF16, "sp")
            ATr = arena.rot(2, [128, 2, 512], BF16, "AT")
            Rr = arena.rot(2, [128, 2, 512], BF16, "R")
            pdr = Rot([(pds[i], [banks[2 * i].b, banks[2 * i + 1].b]) for i in range(3)])
            psO = Rot(banks[6:8])
            m3 = mstrictb.rearrange("p (o c) -> p o c", o=1).to_broadcast([128, 2, 128])
            load_w(wq, sb_w_qkv[j], 8, 0, D)
            load_w(wout, sb_w_out[j], 8, 0, D)
            for g in range(4):
                norm_T([(X[:, 4 * g + k, :], XB[4 * g + k]) for k in range(4)], L * 6 + 0, hT)
                for jc in range(8):
                    bk = psM.next()
                    mm(bk.v[:, :], [(wq.v[:, c, jc * 128:(jc + 1) * 128], hT.v[:, c, :]) for c in range(8)],
                       [wq.b, hT.b], [bk.b])
                    evac(qT.v[:, jc, :], bk.v[:, :], [bk.b], [qT.b], force="dve")
                jmax = 4 * g + 3
                for p in range(8):
                    bkO = psO.next()
                    state = {}

                    def stageA(jk, p=p, g=g, state=state):
                        jj = jk - 4 * g
                        c0 = max(jj, 0) * 128
                        cs = slice(c0, 512)
                        Z, Zb = pdr.next()

                        def fz(e, Z=Z, cs=cs, jk=jk):
                            ins = None
                            for hh in range(2):
                                rows = slice(hh * 64, hh * 64 + 64)
                                ins = e.matmul(Z[:, hh, cs], lhsT=KT.v[rows, p, jk * 128:(jk + 1) * 128],
                                               rhs=qT.v[rows, p, cs], start=True, stop=True,
                                               tile_position=(hh * 64, 0))
                            return ins
                        fw.op(pe, fz, [KT.b, qT.b], Zb)
                        fw.op(act, lambda e, Z=Z, cs=cs: e.activation(out=Z[:, :, cs], in_=Z[:, :, cs], func=AF.Exp),
                              [], Zb)
                        sp_ = spr.next()
                        fw.op(act, lambda e, Z=Z, sp_=sp_, cs=cs: e.activation(out=sp_.v[:, :, cs], in_=Z[:, :, cs],
                                                                             func=AF.Ln, bias=1.0), Zb, [sp_.b])
                        if jj >= 0:
                            fw.op(dve, lambda e, sp_=sp_, c0=c0: e.tensor_tensor(
                                out=sp_.v[:, :, c0:c0 + 128], in0=sp_.v[:, :, c0:c0 + 128], in1=m3, op=ALU.mult),
                                [cstb.b], [sp_.b])
                        state[jk] = (sp_, c0, cs, jj)

                    def stageB(jk, p=p, g=g, state=state, bkO=bkO, jmax=jmax):
                        sp_, c0, cs, jj = state.pop(jk)
                        first = (jk == jmax)
                        last = (jk == 0)
                        Rold = state.get("R")
                        cv = c0 + 128 if jj >= 0 else 0
                        Lp, Lb = pdr.next()

                        def fL(e, Lp=Lp, sp_=sp_, Rold=Rold, cs=cs, c0=c0, cv=cv, jk=jk, jj=jj, first=first):
                            ins = None
                            for hh in range(2):
                                rows = slice(hh * 64, hh * 64 + 64)
                                e.matmul(Lp[:, hh, cs], lhsT=KT.v[rows, p, jk * 128:(jk + 1) * 128],
                                         rhs=qT.v[rows, p, cs], start=True, stop=False, tile_position=(hh * 64, 0))
                                if not first:
                                    e.matmul(Lp[:, hh, cv:512], lhsT=nonesb, rhs=Rold.v[:, hh, cv:512], start=False,
                                             stop=False)
                                if jj >= 0:
                                    e.matmul(Lp[:, hh, c0:c0 + 128], lhsT=identb, rhs=negsb, start=False, stop=False)
                                ins = e.matmul(Lp[:, hh, cs], lhsT=usufb, rhs=sp_.v[:, hh, cs], start=False, stop=True)
                            return ins
                        fw.op(pe, fL, [KT.b, qT.b, sp_.b, cstb.b] + ([Rold.b] if Rold is not None else []), Lb)
                        AT = ATr.next()
                        fw.op(act, lambda e, Lp=Lp, AT=AT, cs=cs: e.activation(out=AT.v[:, :, cs], in_=Lp[:, :, cs],
                                                                             func=AF.Exp), Lb, [AT.b])
                        if not last:
                            Rnew = Rr.next()
                            if first:
                                fw.op(dve, lambda e, Rnew=Rnew, sp_=sp_, cs=cs: e.tensor_copy(
                                    out=Rnew.v[:, :, cs], in_=sp_.v[:, :, cs]), [sp_.b], [Rnew.b])
                            else:
                                fw.op(dve, lambda e, Rnew=Rnew, Rold=Rold, sp_=sp_, cv=cv: e.tensor_tensor(
                                    out=Rnew.v[:, :, cv:512], in0=Rold.v[:, :, cv:512], in1=sp_.v[:, :, cv:512],
                                    op=ALU.add), [sp_.b, Rold.b], [Rnew.b])
                                if jj >= 0:
                                    fw.op(dve, lambda e, Rnew=Rnew, sp_=sp_, c0=c0, cv=cv: e.tensor_copy(
                                        out=Rnew.v[:, :, c0:cv], in_=sp_.v[:, :, c0:cv]), [sp_.b], [Rnew.b])
                            state["R"] = Rnew

                        state["O%d" % jk] = (AT, cs, first, last)

                    def stageC(jk, p=p, state=state, bkO=bkO):
                        AT, cs, first, last = state.pop("O%d" % jk)

                        def fO(e, AT=AT, cs=cs, jk=jk, first=first, last=last, bkO=bkO):
                            ins = None
                            for hh in range(2):
                                h = 2 * p + hh
                                orow = slice(hh * 64, hh * 64 + 64)
                                tp = (0, hh * 64)
                                if first:
                                    e.matmul(bkO.v[orow, :], lhsT=zerosb[:, 0:64], rhs=qT.v[:, p, :], start=True,
                                             stop=False, tile_position=tp)
                                ins = e.matmul(bkO.v[orow, cs], lhsT=V.v[:, jk, h * 64:(h + 1) * 64],
                                               rhs=AT.v[:, hh, cs], start=False, stop=last, tile_position=tp)
                            return ins
                        fw.op(pe, fO, [AT.b, V.b, qT.b, cstb.b], [bkO.b])

                    items = list(range(jmax, -1, -1))
                    n = len(items)
                    for i in range(n + SB_LA + 1):
                        if i < n:
                            stageA(items[i])
                        if SB_LA <= i < n + SB_LA:
                            stageB(items[i - SB_LA])
                        if i >= SB_LA + 1:
                            stageC(items[i - SB_LA - 1])
                    evac(oT.v[:, p, :], bkO.v[:, :], [bkO.b], [oT.b], force="dve")
                for k in range(4):
                    out_proj(oT, slice(k * 128, (k + 1) * 128), wout, 8, 4 * g + k)

        arena_peak = [0]
        for L in layer_ids:
            if "mix" in PHASES:
                if L % 2 == 0:
                    mlstm_phase(L, L // 2)
                else:
                    sb_phase(L, L // 2)
            if "xa" in PHASES:
                xattn_phase(L)
            if "ffn" in PHASES:
                ffn_phase(L)

        fw.dma(sp, out_d.rearrange("(t p) f -> p t f", p=128), X[:, :, :], XB, [])
        for s_ in fw.dma_sems:
            if s_[2] is not None:
                sp.wait(s_[2])
        fw.emit()
    return nc


def _consts():
    c = np.zeros((10, 128, 128), np.float32)
    i = np.arange(128)
    sp_, t = np.meshgrid(i, i, indexing="ij")
    c[0] = np.eye(128)
    c[1] = (sp_ <= t)
    c[2] = np.where(sp_ > t, -30000.0, 0.0)
    c[3] = np.where(sp_ >= t, -1.0, 0.0)
    c[4] = -1.0
    c[5] = np.where(sp_ >= t, -30000.0, 0.0)
    c[6] = (sp_ < t)
    c[7] = 1.0
    c[8] = 0.0
    cf = np.ascontiguousarray(c[[0, 1, 2, 7]].transpose(1, 0, 2))
    cb = np.ascontiguousarray(c[[0, 3, 4, 5, 6, 8]].transpose(1, 0, 2))
    return cf, cb


def _prep_shared(mem_norm_gain, norm_gains, ml_b_gate, ml_head_gain):
    g_all = np.concatenate([norm_gains.reshape(24, D), mem_norm_gain.reshape(1, D)], 0)
    gcol = np.ascontiguousarray(g_all.reshape(25, 8, 128).transpose(2, 0, 1).reshape(128, 200)).astype(np.float32)
    post = norm_gains[:, [1, 3, 5], :].reshape(12, 1, D)
    gbc = np.ascontiguousarray(np.broadcast_to(post, (12, 128, D))).astype(np.float32)
    hgbc = np.ascontiguousarray(np.broadcast_to(ml_head_gain.reshape(2, 1, D), (2, 128, D))).astype(np.float32)
    bgate = np.ascontiguousarray(np.broadcast_to(ml_b_gate.reshape(2, 1, 8), (2, 128, 8))).astype(np.float32)
    return gcol, gbc, hgbc, bgate


_PROG_CACHE = {}


def _get_prog(layer_ids):
    key = tuple(layer_ids)
    if key not in _PROG_CACHE:
        _PROG_CACHE[key] = build_program(list(layer_ids))
    return _PROG_CACHE[key]


LAUNCH_GROUPS = [[0, 1, 2, 3]]


def kernel(x, mem, mem_norm_gain, norm_gains, ml_w_in, ml_b_gate, ml_head_gain, ml_w_out,
           sb_w_qkv, sb_w_out, xa_w_q, xa_w_kv, xa_w_o, ffn_w_gate_up, ffn_w_down):
    f = lambda a: np.ascontiguousarray(np.asarray(a, dtype=np.float32))
    x = f(x)
    mem = f(mem)
    gcol, gbc, hgbc, bgate = _prep_shared(f(mem_norm_gain), f(norm_gains), f(ml_b_gate), f(ml_head_gain))
    shared = {
        "gcol": gcol, "gbc": gbc, "hgbc": hgbc, "bgate": bgate, "constf": _consts()[0], "constb": _consts()[1],
        "ml_w_in": f(ml_w_in), "ml_w_out": f(ml_w_out), "sb_w_qkv": f(sb_w_qkv), "sb_w_out": f(sb_w_out),
        "xa_w_q": f(xa_w_q), "xa_w_kv": f(xa_w_kv), "xa_w_o": f(xa_w_o),
        "ffn_w_gate_up": f(ffn_w_gate_up), "ffn_w_down": f(ffn_w_down),
    }
    cur = [x[b] for b in range(NCORES)]
    for grp in LAUNCH_GROUPS:
        nc = _get_prog(grp)
        in_maps = []
        for b in range(NCORES):
            m = dict(shared)
            m["x"] = cur[b]
            m["mem"] = mem[b]
            in_maps.append(m)
        res = run_bass_kernel_spmd(nc, in_maps, core_ids=list(range(NCORES)))
        cur = [np.asarray(res.results[b]["out"], dtype=np.float32) for b in range(NCORES)]
    return np.stack(cur, 0)
```

```python
import numpy as np
from contextlib import ExitStack
import concourse.bass as bass
import concourse.mybir as mybir
from concourse.bass_utils import run_bass_kernel_spmd

F32 = mybir.dt.float32
BF16 = mybir.dt.bfloat16
AF = mybir.ActivationFunctionType
ALU = mybir.AluOpType
AX = mybir.AxisListType

D = 1024
S = 2048
NT = 16
NMEM = 256
F = 2816
NFC = 22
DEPTH = 4
EPS = 1e-6
NCORES = 8
MLW = 3080
PHASES = ("mix", "xa", "ffn")
SB_LA = 1


class Buf:
    __slots__ = ("name", "w", "r")

    def __init__(self, name=""):
        self.name = name
        self.w = None
        self.r = {}


class Eng:
    def __init__(self, name):
        self.name = name
        self.sem = None
        self.count = 0
        self.epoch = 0
        self.waited = {}
        self.q = []
        self.last = None

    @property
    def key(self):
        return "%s#%d" % (self.name, self.epoch)

    def wait(self, tok):
        if tok is None:
            return
        sem, val, key = tok
        if self.waited.get(key, 0) >= val:
            return
        self.waited[key] = val
        self.q.append(lambda e, sem=sem, val=val: e.wait_ge(sem, val))


class FW:
    NDMA = 16

    def __init__(self, nc, stack):
        self.nc = nc
        self.stack = stack
        self.engs = {}
        for name in ("pe", "act", "dve", "pool", "sp"):
            g = Eng(name)
            g.sem = stack.enter_context(nc.semaphore("sem_" + name))
            self.engs[name] = g
        self.pe, self.act, self.dve, self.pool, self.sp = (
            self.engs[k] for k in ("pe", "act", "dve", "pool", "sp"))
        self.dma_sems = []
        for i in range(self.NDMA):
            s = stack.enter_context(nc.semaphore("dsem%d" % i))
            self.dma_sems.append([s, 0, None])
        self.dma_i = 0
        self.nops = 0

    def _pre(self, eng, reads, writes):
        for b in reads:
            eng.wait(b.w)
        for b in writes:
            eng.wait(b.w)
            for t in b.r.values():
                eng.wait(t)

    def _post(self, tok, reads, writes):
        for b in reads:
            b.r[tok[2]] = tok
        for b in writes:
            b.w = tok
            b.r = {}

    def op(self, eng, fn, reads=(), writes=()):
        self._pre(eng, reads, writes)
        eng.count += 1
        tok = (eng.sem, eng.count, eng.key)
        eng.last = tok
        eng.q.append(lambda e, fn=fn, sem=eng.sem: fn(e).then_inc(sem, 1))
        self._post(tok, reads, writes)
        self.nops += 1
        return tok

    def dma(self, eng, out, in_, reads=(), writes=()):
        self._pre(eng, reads, writes)
        idx = self.dma_i % self.NDMA
        slot = self.dma_sems[idx]
        self.dma_i += 1
        if slot[2] is not None:
            eng.wait(slot[2])
        slot[1] += 16
        tok = (slot[0], slot[1], "dma%d" % idx)
        slot[2] = tok
        eng.q.append(lambda e, out=out, in_=in_, sem=slot[0]: e.dma_start(out=out, in_=in_).then_inc(sem, 16))
        self._post(tok, reads, writes)
        return tok

    def barrier(self):
        toks = [g.last for g in self.engs.values() if g.last is not None]
        toks += [s[2] for s in self.dma_sems if s[2] is not None]
        for g in self.engs.values():
            for t in toks:
                if t[2] != g.key:
                    g.wait(t)
        for g in self.engs.values():
            if g.count > 0:
                g.epoch += 1
                g.sem = self.stack.enter_context(self.nc.semaphore("sem_%s_%d" % (g.name, g.epoch)))
                g.count = 0
                g.last = None

    def emit(self):
        nc = self.nc
        with nc.Block() as block:
            @block.tensor
            def _(e):
                for f in self.pe.q:
                    f(e)

            @block.scalar
            def _(e):
                for f in self.act.q:
                    f(e)

            @block.vector
            def _(e):
                for f in self.dve.q:
                    f(e)

            @block.gpsimd
            def _(e):
                for f in self.pool.q:
                    f(e)

            @block.sync
            def _(e):
                for f in self.sp.q:
                    f(e)


class T:
    __slots__ = ("v", "b")

    def __init__(self, v, b=None, name=""):
        self.v = v
        self.b = b if b is not None else Buf(name)


class Rot:
    def __init__(self, items):
        self.items = items
        self.i = 0

    def next(self):
        it = self.items[self.i % len(self.items)]
        self.i += 1
        return it


class Arena:
    def __init__(self, ap, words):
        self.ap = ap
        self.words = words
        self.off = 0

    def alloc(self, shape, dt, name=""):
        assert shape[0] == 128
        n = 1
        for s in shape[1:]:
            n *= s
        nbytes = n * (2 if dt == BF16 else 4)
        w = (nbytes + 3) // 4
        w = (w + 7) // 8 * 8
        assert self.off + w <= self.words, ("arena overflow", name, self.off, w, self.words)
        v = self.ap[:, self.off:self.off + w]
        self.off += w
        if dt == BF16:
            v = v.bitcast(BF16)
        v = v[:, 0:n]
        if len(shape) == 3:
            v = v.rearrange("p (a b) -> p a b", a=shape[1])
        elif len(shape) == 4:
            v = v.rearrange("p (a b c) -> p a b c", a=shape[1], b=shape[2])
        return T(v, name=name)

    def rot(self, n, shape, dt, name=""):
        return Rot([self.alloc(shape, dt, name + str(i)) for i in range(n)])


def build_program(layer_ids):
    nc = bass.Bass("TRN2", target_bir_lowering=False)
    dr = lambda name, shape: nc.dram_tensor(name, shape, F32, kind="ExternalInput").ap()
    x_in = dr("x", [S, D])
    mem_in = dr("mem", [NMEM, D])
    gcol_in = dr("gcol", [128, 200])
    gbc_in = dr("gbc", [12, 128, D])
    hgbc_in = dr("hgbc", [2, 128, D])
    bgate_in = dr("bgate", [2, 128, 8])
    cstf_in = dr("constf", [128, 4, 128])
    cstb_in = dr("constb", [128, 6, 128])
    ml_w_in = dr("ml_w_in", [2, D, MLW])
    ml_w_out = dr("ml_w_out", [2, D, D])
    sb_w_qkv = dr("sb_w_qkv", [2, D, 3 * D])
    sb_w_out = dr("sb_w_out", [2, D, D])
    xa_w_q = dr("xa_w_q", [4, D, D])
    xa_w_kv = dr("xa_w_kv", [4, D, 2 * D])
    xa_w_o = dr("xa_w_o", [4, D, D])
    ffn_w_gu = dr("ffn_w_gate_up", [4, D, 2 * F])
    ffn_w_d = dr("ffn_w_down", [4, F, D])
    out_d = nc.dram_tensor("out", [S, D], F32, kind="ExternalOutput").ap()

    with ExitStack() as st:
        fw = FW(nc, st)
        pe, act, dve, pool, sp = fw.pe, fw.act, fw.dve, fw.pool, fw.sp
        sbt = lambda name, shape, dt: st.enter_context(nc.sbuf_tensor("sb_" + name, shape, dt))
        pst = lambda name, shape, dt: st.enter_context(nc.psum_tensor(name, shape, dt))

        X = sbt("X", [128, NT, D], F32)[:]
        XB = [Buf("X%d" % i) for i in range(NT)]
        cstf = T(sbt("cstf", [128, 4, 128], F32)[:], name="cstf")
        cstb = T(sbt("cstb", [128, 6, 128], BF16)[:], name="cstb")
        gcol = T(sbt("gcol", [128, 200], F32)[:], name="gcol")
        epsb = T(sbt("epsb", [128, 1], F32)[:], name="epsb")
        gbuf = T(sbt("gbuf", [128, D], F32)[:], name="gbuf")
        colt = sbt("colt", [128, 64], F32)[:]
        cols = Rot([T(colt[:, i:i + 1], name="col%d" % i) for i in range(64)])
        junk = T(sbt("junk", [128, D], BF16)[:], name="junk")
        xn_t = sbt("xn", [128, 2, D], BF16)[:]
        xnr = Rot([T(xn_t[:, i, :], name="xn%d" % i) for i in range(2)])
        tmp_t = sbt("tmpN", [128, 2, 512], F32)[:]
        tmpN = Rot([T(tmp_t[:, i, :], name="tmpN%d" % i) for i in range(2)])
        AW = (nc.sbuf_bytes_remaining - 64) // 4 // 8 * 8
        arena_t = sbt("arena", [128, AW], F32)[:]
        arena = Arena(arena_t, AW)
        pds = [pst("pd%d" % i, [128, 2, 512], F32)[:] for i in range(4)]
        banks = [T(pds[i // 2][:, i % 2, :], name="ps%d" % i) for i in range(8)]
        psM = Rot(banks[0:4])
        psY = Rot(banks[4:6])
        psT = Rot(banks[6:8])
        psY6 = Rot(banks[0:6])

        identf = cstf.v[:, 0, :]
        trif = cstf.v[:, 1, :]
        negmf = cstf.v[:, 2, :]
        onesf = cstf.v[:, 3, :]
        identb = cstb.v[:, 0, :]
        usufb = cstb.v[:, 1, :]
        nonesb = cstb.v[:, 2, :]
        negsb = cstb.v[:, 3, :]
        mstrictb = cstb.v[:, 4, :]
        zerosb = cstb.v[:, 5, :]

        def mm(out_ap, pairs, reads, writes):
            def f(e, out_ap=out_ap, pairs=pairs):
                n = len(pairs)
                ins = None
                for i, (l, r) in enumerate(pairs):
                    ins = e.matmul(out_ap, lhsT=l, rhs=r, start=(i == 0), stop=(i == n - 1))
                return ins
            return fw.op(pe, f, reads, writes)

        def bf16_bank(bank):
            return bank.v.bitcast(BF16).rearrange("p (c f) -> p c f", c=8)

        def transposes(bank, srcs, reads):
            pv = bf16_bank(bank)

            def f(e, pv=pv, srcs=srcs):
                ins = None
                for i, s_ in enumerate(srcs):
                    ins = e.transpose(out=pv[:, i, :], in_=s_, identity=identb)
                return ins
            fw.op(pe, f, list(reads) + [cstb.b], [bank.b])
            return pv

        def rstd_from(ssum, scale):
            r = cols.next()
            fw.op(act, lambda e: e.activation(out=r.v, in_=ssum.v, func=AF.Ln, scale=scale, bias=epsb.v[:, 0:1]),
                  [ssum.b, epsb.b], [r.b])
            fw.op(act, lambda e: e.activation(out=r.v, in_=r.v, func=AF.Exp, scale=-0.5), [], [r.b])
            return r

        def norm_T(srcs, gi, hT, evac_alt=0):
            for k, (sv, sbuf_) in enumerate(srcs):
                s_ = cols.next()
                fw.op(act, lambda e, sv=sv, s_=s_: e.activation(out=junk.v, in_=sv, func=AF.Square, accum_out=s_.v),
                      [sbuf_], [junk.b, s_.b])
                r = rstd_from(s_, 1.0 / D)
                xn = xnr.next()
                fw.op(dve, lambda e, sv=sv, xn=xn, r=r: e.tensor_scalar(out=xn.v, in0=sv, scalar1=r.v, scalar2=None,
                                                                        op0=ALU.mult),
                      [sbuf_, r.b], [xn.b])
                bank = psT.next()
                pv = transposes(bank, [xn.v[:, c * 128:(c + 1) * 128] for c in range(8)], [xn.b])
                g3 = gcol.v[:, gi * 8:(gi + 1) * 8].rearrange("p (c o) -> p c o", o=1).to_broadcast([128, 8, 128])
                fw.op(dve, lambda e, pv=pv, k=k, g3=g3: e.tensor_tensor(out=hT.v[:, :, k * 128:(k + 1) * 128], in0=pv,
                                                                       in1=g3, op=ALU.mult),
                      [bank.b, gcol.b], [hT.b])

        def post_norm(tt, y0, y1):
            s0 = cols.next()
            s1 = cols.next()
            fw.op(act, lambda e: e.activation(out=junk.v[:, 0:512], in_=y0.v[:, :], func=AF.Square, accum_out=s0.v),
                  [y0.b], [junk.b, s0.b])
            fw.op(act, lambda e: e.activation(out=junk.v[:, 512:1024], in_=y1.v[:, :], func=AF.Square, accum_out=s1.v),
                  [y1.b], [junk.b, s1.b])
            ssum = cols.next()
            fw.op(dve, lambda e: e.tensor_tensor(out=ssum.v, in0=s0.v, in1=s1.v, op=ALU.add), [s0.b, s1.b], [ssum.b])
            r = rstd_from(ssum, 1.0 / D)
            for h, y in enumerate((y0, y1)):
                t = tmpN.next()
                fw.op(dve, lambda e, y=y, t=t, h=h: e.scalar_tensor_tensor(
                    out=t.v, in0=y.v[:, :], scalar=r.v, in1=gbuf.v[:, h * 512:(h + 1) * 512], op0=ALU.mult, op1=ALU.mult),
                    [y.b, r.b, gbuf.b], [t.b])
                fw.op(pool, lambda e, t=t, h=h: e.tensor_tensor(
                    out=X[:, tt, h * 512:(h + 1) * 512], in0=X[:, tt, h * 512:(h + 1) * 512], in1=t.v, op=ALU.add),
                    [t.b], [XB[tt]])

        def load_w(dst, src2d, nchunk, f0, f1, step=1024):
            srcv = src2d.rearrange("(c p) f -> p c f", p=128)
            for a in range(f0, f1, step):
                b = min(a + step, f1)
                fw.dma(pool, dst.v[:, :, a - f0:b - f0], srcv[:, :, a:b], [], [dst.b])

        def out_proj(inT, kslice, w, nk, tt, psY=psY):
            ys = []
            for half in range(2):
                y = psY.next()
                mm(y.v[:, :], [(inT.v[:, c, kslice], w.v[:, c, half * 512:(half + 1) * 512]) for c in range(nk)],
                   [inT.b, w.b], [y.b])
                ys.append(y)
            post_norm(tt, ys[0], ys[1])

        evac_ctr = [0]

        def evac(out_ap, in_ap, reads, writes, scale=None, force=None):
            evac_ctr[0] += 1
            if (evac_ctr[0] % 2 == 0 and force is None) or force == "act":
                if scale is None:
                    fw.op(act, lambda e: e.copy(out=out_ap, in_=in_ap), reads, writes)
                else:
                    fw.op(act, lambda e: e.mul(out=out_ap, in_=in_ap, mul=scale), reads, writes)
            else:
                if scale is None:
                    fw.op(dve, lambda e: e.tensor_copy(out=out_ap, in_=in_ap), reads, writes)
                else:
                    fw.op(dve, lambda e: e.tensor_scalar(out=out_ap, in0=in_ap, scalar1=scale, scalar2=None,
                                                         op0=ALU.mult), reads, writes)

        def load_gain(idx):
            fw.dma(sp, gbuf.v, gbc_in[idx], [], [gbuf.b])

        fw.dma(sp, X[:, :, :], x_in.rearrange("(t p) f -> p t f", p=128), [], XB)
        fw.dma(sp, cstf.v, cstf_in, [], [cstf.b])
        fw.dma(pool, cstb.v, cstb_in, [], [cstb.b])
        fw.dma(sp, gcol.v, gcol_in, [], [gcol.b])
        fw.op(pool, lambda e: e.memset(epsb.v, EPS), [], [epsb.b])

        def xattn_phase(L):
            fw.barrier()
            arena.off = 0
            wq = arena.alloc([128, 8, D], BF16, "wq")
            wo = arena.alloc([128, 8, D], BF16, "wo")
            kmT = arena.alloc([128, 8, NMEM], BF16, "kmT")
            vm = arena.alloc([128, 2, D], BF16, "vm")
            mark = arena.off
            wkv = arena.alloc([128, 8, 2 * D], BF16, "wkv")
            memt = arena.alloc([128, 2, D], F32, "memt")
            memnT = arena.alloc([128, 8, NMEM], BF16, "memnT")
            load_gain(L * 3 + 1)
            fw.dma(sp, memt.v, mem_in.rearrange("(t p) f -> p t f", p=128), [], [memt.b])
            load_w(wkv, xa_w_kv[L], 8, 0, 2 * D)
            load_w(wq, xa_w_q[L], 8, 0, D)
            load_w(wo, xa_w_o[L], 8, 0, D)
            norm_T([(memt.v[:, i, :], memt.b) for i in range(2)], 24, memnT)
            for jc in range(8):
                bk = psM.next()
                mm(bk.v[:, 0:NMEM], [(wkv.v[:, c, jc * 128:(jc + 1) * 128], memnT.v[:, c, :]) for c in range(8)],
                   [wkv.b, memnT.b], [bk.b])
                evac(kmT.v[:, jc, :], bk.v[:, 0:NMEM], [bk.b], [kmT.b])
            for mt in range(2):
                for half in range(2):
                    bk = psM.next()
                    mm(bk.v[:, :], [(memnT.v[:, c, mt * 128:(mt + 1) * 128],
                                     wkv.v[:, c, D + half * 512:D + (half + 1) * 512]) for c in range(8)],
                       [wkv.b, memnT.b], [bk.b])
                    evac(vm.v[:, mt, half * 512:(half + 1) * 512], bk.v[:, :], [bk.b], [vm.b])
            fw.barrier()
            arena.off = mark
            hTr = arena.rot(2, [128, 8, 512], BF16, "hT")
            qTr = arena.rot(2, [128, 8, 512], BF16, "qT")
            Pr = arena.rot(3, [128, 4, NMEM], BF16, "P")
            PTr = arena.rot(2, [128, 8, 128], BF16, "PT")
            otr = arena.rot(3, [128, D], BF16, "otok")
            oTr = arena.rot(2, [128, 8, 128], BF16, "oT")
            st4 = arena.rot(4, [128, 16], F32, "st4")
            sc = 1.0 / 16.0
            ctx = {}

            def prep(g):
                hT = hTr.next()
                qT = qTr.next()
                norm_T([(X[:, 4 * g + k, :], XB[4 * g + k]) for k in range(4)], L * 6 + 2, hT)
                for jc in range(8):
                    bk = psM.next()
                    mm(bk.v[:, :], [(wq.v[:, c, jc * 128:(jc + 1) * 128], hT.v[:, c, :]) for c in range(8)],
                       [wq.b, hT.b], [bk.b])
                    evac(qT.v[:, jc, :], bk.v[:, :], [bk.b], [qT.b])
                ctx["qT%d" % g] = qT

            def s0(tt):
                g, k = tt // 4, tt % 4
                if tt == 0:
                    prep(0)
                if k == 2 and g + 1 < 4:
                    prep(g + 1)
                qT = ctx["qT%d" % g]
                ks = slice(k * 128, (k + 1) * 128)
                P = Pr.next()
                s4 = st4.next()
                for hp in range(2):
                    bk = psM.next()

                    def fsc(e, bk=bk, hp=hp, ks=ks, qT=qT):
                        ins = None
                        for hh in range(2):
                            h = 2 * hp + hh
                            for dc in range(2):
                                ins = e.matmul(bk.v[:, hh * NMEM:(hh + 1) * NMEM], lhsT=qT.v[:, 2 * h + dc, ks],
                                               rhs=kmT.v[:, 2 * h + dc, :], start=(dc == 0), stop=(dc == 1))
                        return ins
                    fw.op(pe, fsc, [qT.b, kmT.b], [bk.b])
                    fw.op(dve, lambda e, bk=bk, hp=hp, s4=s4: e.tensor_reduce(
                        out=s4.v[:, 2 * hp:2 * hp + 2], in_=bk.v[:, :].rearrange("p (h m) -> p h m", h=2),
                        axis=AX.X, op=ALU.max), [bk.b], [s4.b])
                    fw.op(dve, lambda e, hp=hp, s4=s4: e.tensor_scalar(
                        out=s4.v[:, 2 * hp:2 * hp + 2], in0=s4.v[:, 2 * hp:2 * hp + 2], scalar1=-sc, scalar2=None,
                        op0=ALU.mult), [], [s4.b])
                    for hh in range(2):
                        h = 2 * hp + hh
                        fw.op(act, lambda e, bk=bk, hh=hh, h=h, P=P, s4=s4: e.activation(
                            out=P.v[:, h, :], in_=bk.v[:, hh * NMEM:(hh + 1) * NMEM], func=AF.Exp, scale=sc,
                            bias=s4.v[:, h:h + 1], accum_out=s4.v[:, 4 + h:5 + h]), [bk.b, s4.b], [P.b, s4.b])
                fw.op(act, lambda e, s4=s4: e.activation(out=s4.v[:, 8:12], in_=s4.v[:, 4:8], func=AF.Ln),
                      [s4.b], [s4.b])
                fw.op(act, lambda e, s4=s4: e.activation(out=s4.v[:, 8:12], in_=s4.v[:, 8:12], func=AF.Exp,
                                                        scale=-1.0), [], [s4.b])
                ctx["P%d" % tt] = (P, s4)

            def s1(tt):
                P, s4 = ctx.pop("P%d" % tt)
                bkT = psT.next()
                pv = transposes(bkT, [P.v[:, h, mt * 128:(mt + 1) * 128] for h in range(4) for mt in range(2)],
                                [P.b])
                PT = PTr.next()
                evac(PT.v, pv, [bkT.b], [PT.b])
                ot = otr.next()
                for hp in range(2):
                    bk = psM.next()

                    def fpv(e, bk=bk, hp=hp, PT=PT):
                        ins = None
                        for hh in range(2):
                            h = 2 * hp + hh
                            for mt in range(2):
                                ins = e.matmul(bk.v[:, hh * 256:(hh + 1) * 256], lhsT=PT.v[:, 2 * h + mt, :],
                                               rhs=vm.v[:, mt, h * 256:(h + 1) * 256], start=(mt == 0),
                                               stop=(mt == 1))
                        return ins
                    fw.op(pe, fpv, [PT.b, vm.b], [bk.b])
                    rb = s4.v[:, 8 + 2 * hp:10 + 2 * hp].rearrange("p (h o) -> p h o", o=1).to_broadcast(
                        [128, 2, 256])
                    fw.op(dve, lambda e, bk=bk, hp=hp, ot=ot, rb=rb: e.tensor_tensor(
                        out=ot.v[:, hp * 512:(hp + 1) * 512].rearrange("p (h v) -> p h v", h=2),
                        in0=bk.v[:, :].rearrange("p (h v) -> p h v", h=2), in1=rb, op=ALU.mult),
                        [bk.b, s4.b], [ot.b])
                ctx["ot%d" % tt] = ot

            def s2(tt):
                ot = ctx.pop("ot%d" % tt)
                bkT = psT.next()
                pv = transposes(bkT, [ot.v[:, c * 128:(c + 1) * 128] for c in range(8)], [ot.b])
                oT = oTr.next()
                evac(oT.v, pv, [bkT.b], [oT.b])
                out_proj(oT, slice(0, 128), wo, 8, tt)

            stages = [(s0, 0), (s1, 2), (s2, 4)]
            for step in range(NT + 4):
                for fn_, off in reversed(stages):
                    tt = step - off
                    if 0 <= tt < NT:
                        fn_(tt)

        def ffn_phase(L):
            fw.barrier()
            arena.off = 0
            wd = arena.alloc([128, NFC, D], BF16, "wd")
            actT = arena.alloc([128, NFC, 1024], BF16, "actT")
            hT = arena.alloc([128, 8, 1024], BF16, "hT")
            pcs = arena.rot(2, [128, 8, 2, 256], BF16, "pc")
            stmp = tmpN
            load_gain(L * 3 + 2)
            wdv = ffn_w_d[L].rearrange("(j p) f -> p j f", p=128)
            for a in range(0, NFC, 6):
                b = min(a + 6, NFC)
                fw.dma(pool, wd.v[:, a:b, :], wdv[:, a:b, :], [], [wd.b])
            wguv = ffn_w_gu[L].rearrange("(c p) f -> p c f", p=128)
            norm_T([(X[:, k, :], XB[k]) for k in range(8)], L * 6 + 4, hT)
            for G in range(2):
                for jp in range(NFC // 2):
                    pc = pcs.next()
                    fw.dma(pool, pc.v[:, :, 0, :], wguv[:, :, jp * 256:(jp + 1) * 256], [], [pc.b])
                    fw.dma(pool, pc.v[:, :, 1, :], wguv[:, :, F + jp * 256:F + (jp + 1) * 256], [], [pc.b])
                    for jj in range(2):
                        j = 2 * jp + jj
                        for hf in range(2):
                            ts = slice(hf * 512, (hf + 1) * 512)
                            bg_ = psM.next()
                            mm(bg_.v[:, :], [(pc.v[:, c, 0, jj * 128:(jj + 1) * 128], hT.v[:, c, ts]) for c in range(8)],
                               [pc.b, hT.b], [bg_.b])
                            bu_ = psM.next()
                            mm(bu_.v[:, :], [(pc.v[:, c, 1, jj * 128:(jj + 1) * 128], hT.v[:, c, ts]) for c in range(8)],
                               [pc.b, hT.b], [bu_.b])
                            sg = stmp.next()
                            fw.op(act, lambda e, bg_=bg_, sg=sg: e.activation(out=sg.v, in_=bg_.v[:, :], func=AF.Silu),
                                  [bg_.b], [sg.b])
                            fw.op(dve, lambda e, bu_=bu_, sg=sg, j=j, ts=ts: e.tensor_tensor(
                                out=actT.v[:, j, ts], in0=bu_.v[:, :], in1=sg.v, op=ALU.mult), [bu_.b, sg.b], [actT.b])
                if G == 0:
                    norm_T([(X[:, 8 + k, :], XB[8 + k]) for k in range(8)], L * 6 + 4, hT)
                for k in range(8):
                    out_proj(actT, slice(k * 128, (k + 1) * 128), wd, NFC, 8 * G + k, psY=psY6)

        def mlstm_phase(L, j):
            fw.barrier()
            arena.off = 0
            win = arena.alloc([128, 8, MLW], BF16, "win")
            wout = arena.alloc([128, 8, D], BF16, "wout")
            hg = arena.alloc([128, D], F32, "hg")
            bgt = arena.alloc([128, 8], F32, "bg")
            hT = arena.alloc([128, 8, 512], BF16, "hT")
            qtr = arena.rot(2, [128, 512], BF16, "qtok")
            ktr = arena.rot(2, [128, 512], BF16, "ktok")
            var = arena.rot(2, [128, 4, 264], BF16, "vaug")
            ogr = arena.rot(2, [128, D], F32, "og")
            qkTr = arena.rot(2, [128, 8, 128], BF16, "qkT")
            Kwr = arena.rot(2, [128, 4, 128], BF16, "Kw")
            g8r = arena.rot(2, [128, 40], F32, "g8")
            Dtr = arena.rot(2, [128, 128], F32, "Dt")
            Str = arena.rot(2, [128, 128], BF16, "St")
            isbr = arena.rot(2, [128, 264], F32, "isb")
            Hsr = arena.rot(2, [128, 264], F32, "Hs")
            C32 = [arena.alloc([128, 264], F32, "C32_%d" % h) for h in range(4)]
            Cb = [arena.alloc([128, 264], BF16, "Cb_%d" % h) for h in range(4)]
            mor = arena.rot(2, [128, D], BF16, "mo")
            moTr = arena.rot(2, [128, 8, 128], BF16, "moT")
            load_gain(L * 3 + 0)
            load_w(win, ml_w_in[j], 8, 0, MLW)
            load_w(wout, ml_w_out[j], 8, 0, D)
            fw.dma(sp, hg.v, hgbc_in[j], [], [hg.b])
            fw.dma(sp, bgt.v, bgate_in[j], [], [bgt.b])
            for h in range(4):
                fw.op(pool, lambda e, h=h: e.memset(C32[h].v, 0.0), [], [C32[h].b])
                fw.op(pool, lambda e, h=h: e.memset(Cb[h].v, 0.0), [], [Cb[h].b])
            for va in var.items:
                fw.op(pool, lambda e, va=va: e.memset(va.v, 1.0), [], [va.b])
            KSC = 128.0 ** -0.5
            mctx = {}

            def ms0(tt):
                g, k = tt // 4, tt % 4
                if k == 0:
                    norm_T([(X[:, 4 * g + kk, :], XB[4 * g + kk]) for kk in range(4)], L * 6 + 0, hT)
                ks = slice(k * 128, (k + 1) * 128)

                def proj(c0, c1):
                    bk = psM.next()
                    mm(bk.v[:, 0:c1 - c0], [(hT.v[:, c, ks], win.v[:, c, c0:c1]) for c in range(8)],
                       [hT.b, win.b], [bk.b])
                    return bk
                qt = qtr.next()
                kt = ktr.next()
                va = var.next()
                og = ogr.next()
                g8 = g8r.next()
                bk = proj(0, 512)
                fw.op(act, lambda e, bk=bk, qt=qt: e.copy(out=qt.v, in_=bk.v[:, :]), [bk.b], [qt.b])
                bk = proj(512, 1024)
                fw.op(dve, lambda e, bk=bk, kt=kt: e.tensor_scalar(out=kt.v, in0=bk.v[:, :], scalar1=KSC,
                                                                   scalar2=None, op0=ALU.mult), [bk.b], [kt.b])
                for i in range(2):
                    bk = proj(1024 + i * 512, 1536 + i * 512)
                    evac(va.v[:, 2 * i:2 * i + 2, 0:256], bk.v[:, :].rearrange("p (h v) -> p h v", h=2),
                         [bk.b], [va.b])
                for i in range(2):
                    bk = proj(2048 + i * 512, 2560 + i * 512)
                    fw.op(act, lambda e, bk=bk, og=og, i=i: e.activation(out=og.v[:, i * 512:(i + 1) * 512],
                                                                        in_=bk.v[:, :], func=AF.Sigmoid),
                          [bk.b], [og.b])
                fw.op(pool, lambda e, og=og: e.tensor_tensor(out=og.v, in0=og.v, in1=hg.v, op=ALU.mult),
                      [hg.b], [og.b])
                bk = proj(3072, 3080)
                fw.op(dve, lambda e, bk=bk, g8=g8: e.tensor_tensor(out=g8.v[:, 0:8], in0=bk.v[:, 0:8], in1=bgt.v,
                                                                  op=ALU.add), [bk.b, bgt.b], [g8.b])
                fw.op(act, lambda e, g8=g8: e.activation(out=g8.v[:, 0:8], in_=g8.v[:, 0:8], func=AF.Tanh,
                                                        scale=1.0 / 15.0), [], [g8.b])
                fw.op(dve, lambda e, g8=g8: e.tensor_scalar(out=g8.v[:, 0:8], in0=g8.v[:, 0:8], scalar1=15.0,
                                                           scalar2=None, op0=ALU.mult), [], [g8.b])
                fw.op(act, lambda e, g8=g8: e.activation(out=g8.v[:, 8:12], in_=g8.v[:, 4:8], func=AF.Exp,
                                                        scale=-1.0), [], [g8.b])
                fw.op(act, lambda e, g8=g8: e.activation(out=g8.v[:, 8:12], in_=g8.v[:, 8:12], func=AF.Ln,
                                                        bias=1.0), [], [g8.b])
                fw.op(dve, lambda e, g8=g8: e.tensor_scalar(out=g8.v[:, 8:12], in0=g8.v[:, 8:12], scalar1=-1.0,
                                                           scalar2=None, op0=ALU.mult), [], [g8.b])
                bkc = psT.next()

                def fcs(e, bkc=bkc, g8=g8):
                    e.matmul(bkc.v[:, 0:4], lhsT=trif, rhs=g8.v[:, 8:12], start=True, stop=True)
                    return e.matmul(bkc.v[:, 4:8], lhsT=onesf, rhs=g8.v[:, 8:12], start=True, stop=True)
                fw.op(pe, fcs, [g8.b, cstf.b], [bkc.b])
                fw.op(dve, lambda e, bkc=bkc, g8=g8: e.tensor_copy(out=g8.v[:, 28:36], in_=bkc.v[:, 0:8]),
                      [bkc.b], [g8.b])
                fw.op(dve, lambda e, g8=g8: e.tensor_tensor(out=g8.v[:, 12:16], in0=g8.v[:, 0:4],
                                                           in1=g8.v[:, 28:32], op=ALU.subtract), [], [g8.b])
                fw.op(dve, lambda e, g8=g8: e.tensor_tensor(out=g8.v[:, 20:24], in0=g8.v[:, 12:16],
                                                           in1=g8.v[:, 32:36], op=ALU.add), [], [g8.b])
                fw.op(dve, lambda e, g8=g8: e.tensor_copy(out=g8.v[:, 24:28], in_=g8.v[:, 32:36]), [], [g8.b])
                fw.op(act, lambda e, g8=g8: e.activation(out=g8.v[:, 16:20], in_=g8.v[:, 28:32], func=AF.Exp),
                      [], [g8.b])
                fw.op(act, lambda e, g8=g8: e.activation(out=g8.v[:, 20:28], in_=g8.v[:, 20:28], func=AF.Exp),
                      [], [g8.b])
                bkT = psT.next()
                pv = transposes(bkT, [qt.v[:, h * 128:(h + 1) * 128] for h in range(4)] +
                                [kt.v[:, h * 128:(h + 1) * 128] for h in range(4)], [qt.b, kt.b])
                qkT = qkTr.next()
                evac(qkT.v, pv, [bkT.b], [qkT.b])
                Kw = Kwr.next()
                wb = g8.v[:, 20:24].rearrange("p (h o) -> p h o", o=1).to_broadcast([128, 4, 128])
                fw.op(dve, lambda e, Kw=Kw, kt=kt, wb=wb: e.tensor_tensor(
                    out=Kw.v, in0=kt.v.rearrange("p (h d) -> p h d", h=4), in1=wb, op=ALU.mult),
                    [kt.b, g8.b], [Kw.b])
                mctx["a%d" % tt] = (va, og, g8, qkT, Kw)

            def ms1(tt):
                va, og, g8, qkT, Kw = mctx.pop("a%d" % tt)
                mo = mor.next()
                def hgen(h):
                    bkA = psM.next()

                    def fA(e, bkA=bkA, g8=g8, qkT=qkT, h=h):
                        e.matmul(bkA.v[:, 0:128], lhsT=g8.v[:, 8 + h:9 + h].to_broadcast([128, 128]), rhs=trif,
                                 start=True, stop=False)
                        e.matmul(bkA.v[:, 0:128], lhsT=identf, rhs=negmf, start=False, stop=True)
                        return e.matmul(bkA.v[:, 128:256], lhsT=qkT.v[:, 4 + h, :], rhs=qkT.v[:, h, :],
                                        start=True, stop=True)
                    fw.op(pe, fA, [g8.b, qkT.b, cstf.b], [bkA.b])
                    yield
                    Dt = Dtr.next()
                    fw.op(act, lambda e, bkA=bkA, Dt=Dt, g8=g8, h=h: e.activation(
                        out=Dt.v, in_=bkA.v[:, 0:128], func=AF.Exp, bias=g8.v[:, 12 + h:13 + h]),
                        [bkA.b, g8.b], [Dt.b])
                    yield
                    St = Str.next()
                    fw.op(dve, lambda e, bkA=bkA, Dt=Dt, St=St: e.tensor_tensor(
                        out=St.v, in0=bkA.v[:, 128:256], in1=Dt.v, op=ALU.mult), [bkA.b, Dt.b], [St.b])
                    yield
                    bkI = psM.next()
                    mm(bkI.v[:, 0:257], [(St.v, va.v[:, h, 0:257])], [St.b, va.b], [bkI.b])
                    yield
                    bkN = psM.next()
                    mm(bkN.v[:, 0:257], [(qkT.v[:, h, :], Cb[h].v[:, 0:257])], [qkT.b, Cb[h].b], [bkN.b])
                    yield
                    isb = isbr.next()
                    fw.op(act, lambda e, bkN=bkN, isb=isb, g8=g8, h=h: e.activation(
                        out=isb.v[:, 0:257], in_=bkN.v[:, 0:257], func=AF.Copy, scale=g8.v[:, 16 + h:17 + h]),
                        [bkN.b, g8.b], [isb.b])
                    yield
                    Hs = Hsr.next()
                    fw.op(dve, lambda e, bkI=bkI, isb=isb, Hs=Hs: e.tensor_tensor(
                        out=Hs.v[:, 0:257], in0=bkI.v[:, 0:257], in1=isb.v[:, 0:257], op=ALU.add),
                        [bkI.b, isb.b], [Hs.b])
                    yield
                    c1 = cols.next()
                    fw.op(dve, lambda e, Hs=Hs, c1=c1: e.tensor_scalar(
                        out=c1.v, in0=Hs.v[:, 256:257], scalar1=-1.0, scalar2=None, op0=ALU.mult),
                        [Hs.b], [c1.b])
                    yield
                    fw.op(dve, lambda e, Hs=Hs, c1=c1: e.tensor_tensor(
                        out=c1.v, in0=c1.v, in1=Hs.v[:, 256:257], op=ALU.max), [Hs.b], [c1.b])
                    yield
                    fw.op(dve, lambda e, c1=c1: e.tensor_scalar(
                        out=c1.v, in0=c1.v, scalar1=1.0, scalar2=None, op0=ALU.max), [], [c1.b])
                    yield
                    fw.op(act, lambda e, c1=c1: e.activation(out=c1.v, in_=c1.v, func=AF.Ln), [c1.b], [c1.b])
                    yield
                    fw.op(act, lambda e, c1=c1: e.activation(out=c1.v, in_=c1.v, func=AF.Exp, scale=-1.0),
                          [], [c1.b])
                    yield
                    ssh = cols.next()
                    fw.op(act, lambda e, Hs=Hs, c1=c1, ssh=ssh: e.activation(
                        out=junk.v[:, 0:256], in_=Hs.v[:, 0:256], func=AF.Square, scale=c1.v, accum_out=ssh.v),
                        [Hs.b, c1.b], [junk.b, ssh.b])
                    yield
                    r = rstd_from(ssh, 1.0 / 256.0)
                    yield
                    fw.op(dve, lambda e, r=r, c1=c1: e.tensor_tensor(out=r.v, in0=r.v, in1=c1.v, op=ALU.mult),
                          [c1.b], [r.b])
                    yield
                    fw.op(dve, lambda e, Hs=Hs, r=r, og=og, mo=mo, h=h: e.scalar_tensor_tensor(
                        out=mo.v[:, h * 256:(h + 1) * 256], in0=Hs.v[:, 0:256], scalar=r.v,
                        in1=og.v[:, h * 256:(h + 1) * 256], op0=ALU.mult, op1=ALU.mult),
                        [Hs.b, r.b, og.b], [mo.b])
                    yield
                    bkU = psM.next()
                    mm(bkU.v[:, 0:257], [(Kw.v[:, h, :], va.v[:, h, 0:257])], [Kw.b, va.b], [bkU.b])
                    yield
                    fw.op(dve, lambda e, bkU=bkU, g8=g8, h=h: e.scalar_tensor_tensor(
                        out=C32[h].v[:, 0:257], in0=C32[h].v[:, 0:257], scalar=g8.v[:, 24 + h:25 + h],
                        in1=bkU.v[:, 0:257], op0=ALU.mult, op1=ALU.add), [bkU.b, g8.b], [C32[h].b])
                    yield
                    fw.op(pool, lambda e, h=h: e.tensor_copy(out=Cb[h].v[:, 0:257], in_=C32[h].v[:, 0:257]),
                          [C32[h].b], [Cb[h].b])
                    yield

                for pair in ((0, 1), (2, 3)):
                    gens = [hgen(h) for h in pair]
                    while gens:
                        for g_ in list(gens):
                            try:
                                next(g_)
                            except StopIteration:
                                gens.remove(g_)
                mctx["m%d" % tt] = mo

            def ms2(tt):
                mo = mctx.pop("m%d" % tt)
                bkT = psT.next()
                pv = transposes(bkT, [mo.v[:, c * 128:(c + 1) * 128] for c in range(8)], [mo.b])
                moT = moTr.next()
                evac(moT.v, pv, [bkT.b], [moT.b])
                out_proj(moT, slice(0, 128), wout, 8, tt)

            mstages = [ms0, ms1, ms2]
            for step in range(NT + len(mstages) - 1):
                for si in reversed(range(len(mstages))):
                    tt = step - si
                    if 0 <= tt < NT:
                        mstages[si](tt)

        def sb_phase(L, j):
            fw.barrier()
            arena.off = 0
            KT = arena.alloc([128, 8, S], BF16, "KT")
            V = arena.alloc([128, NT, D], BF16, "V")
            mark = arena.off
            wkv = arena.alloc([128, 8, 2 * D], BF16, "wkv")
            hTs = [arena.alloc([128, 8, 512], BF16, "hTa"), arena.alloc([128, 8, 512], BF16, "hTb")]
            load_gain(L * 3 + 0)
            load_w(wkv, sb_w_qkv[j], 8, D, 3 * D)
            norm_T([(X[:, k, :], XB[k]) for k in range(4)], L * 6 + 0, hTs[0])
            for g in range(4):
                hT = hTs[g % 2]
                if g + 1 < 4:
                    norm_T([(X[:, 4 * (g + 1) + k, :], XB[4 * (g + 1) + k]) for k in range(4)], L * 6 + 0,
                           hTs[(g + 1) % 2])
                for jc in range(8):
                    bk = psM.next()
                    mm(bk.v[:, :], [(wkv.v[:, c, jc * 128:(jc + 1) * 128], hT.v[:, c, :]) for c in range(8)],
                       [wkv.b, hT.b], [bk.b])
                    evac(KT.v[:, jc, g * 512:(g + 1) * 512], bk.v[:, :], [bk.b], [KT.b], scale=0.125)
                for k in range(4):
                    for half in range(2):
                        bk = psM.next()
                        mm(bk.v[:, :], [(hT.v[:, c, k * 128:(k + 1) * 128],
                                         wkv.v[:, c, D + half * 512:D + (half + 1) * 512]) for c in range(8)],
                           [wkv.b, hT.b], [bk.b])
                        evac(V.v[:, 4 * g + k, half * 512:(half + 1) * 512], bk.v[:, :], [bk.b], [V.b])
            fw.barrier()
            arena.off = mark
            wq = arena.alloc([128, 8, D], BF16, "wq")
            wout = arena.alloc([128, 8, D], BF16, "wout")
            hT = arena.alloc([128, 8, 512], BF16, "hT2")
            oT = hT
            qT = arena.alloc([128, 8, 512], BF16, "qT")
            spr = arena.rot(2, [128, 2, 512], BF16, "sp")
            ATr = arena.rot(2, [128, 2, 512], BF16, "AT")
            Rr = arena.rot(2, [128, 2, 512], BF16, "R")
            pdr = Rot([(pds[i], [banks[2 * i].b, banks[2 * i + 1].b]) for i in range(3)])
            psO = Rot(banks[6:8])
            m3 = mstrictb.rearrange("p (o c) -> p o c", o=1).to_broadcast([128, 2, 128])
            load_w(wq, sb_w_qkv[j], 8, 0, D)
            load_w(wout, sb_w_out[j], 8, 0, D)
            for g in range(4):
                norm_T([(X[:, 4 * g + k, :], XB[4 * g + k]) for k in range(4)], L * 6 + 0, hT)
                for jc in range(8):
                    bk = psM.next()
                    mm(bk.v[:, :], [(wq.v[:, c, jc * 128:(jc + 1) * 128], hT.v[:, c, :]) for c in range(8)],
                       [wq.b, hT.b], [bk.b])
                    evac(qT.v[:, jc, :], bk.v[:, :], [bk.b], [qT.b], force="dve")
                jmax = 4 * g + 3
                seq = []
                for p in range(8):
                    bkO = psO.next()
                    state = {}

                    def stageA(jk, p=p, g=g, state=state):
                        jj = jk - 4 * g
                        c0 = max(jj, 0) * 128
                        cs = slice(c0, 512)
                        Z, Zb = pdr.next()

                        def fz(e, Z=Z, cs=cs, jk=jk):
                            ins = None
                            for hh in range(2):
                                rows = slice(hh * 64, hh * 64 + 64)
                                ins = e.matmul(Z[:, hh, cs], lhsT=KT.v[rows, p, jk * 128:(jk + 1) * 128],
                                               rhs=qT.v[rows, p, cs], start=True, stop=True,
                                               tile_position=(hh * 64, 0))
                            return ins
                        fw.op(pe, fz, [KT.b, qT.b], Zb)
                        fw.op(act, lambda e, Z=Z, cs=cs: e.activation(out=Z[:, :, cs], in_=Z[:, :, cs], func=AF.Exp),
                              [], Zb)
                        sp_ = spr.next()
                        fw.op(act, lambda e, Z=Z, sp_=sp_, cs=cs: e.activation(out=sp_.v[:, :, cs], in_=Z[:, :, cs],
                                                                             func=AF.Ln, bias=1.0), Zb, [sp_.b])
                        if jj >= 0:
                            fw.op(dve, lambda e, sp_=sp_, c0=c0: e.tensor_tensor(
                                out=sp_.v[:, :, c0:c0 + 128], in0=sp_.v[:, :, c0:c0 + 128], in1=m3, op=ALU.mult),
                                [cstb.b], [sp_.b])
                        state[jk] = (sp_, c0, cs, jj)

                    def stageB(jk, p=p, g=g, state=state, bkO=bkO, jmax=jmax):
                        sp_, c0, cs, jj = state.pop(jk)
                        first = (jk == jmax)
                        last = (jk == 0)
                        Rold = state.get("R")
                        cv = c0 + 128 if jj >= 0 else 0
                        Lp, Lb = pdr.next()

                        def fL(e, Lp=Lp, sp_=sp_, Rold=Rold, cs=cs, c0=c0, cv=cv, jk=jk, jj=jj, first=first):
                            ins = None
                            for hh in range(2):
                                rows = slice(hh * 64, hh * 64 + 64)
                                e.matmul(Lp[:, hh, cs], lhsT=KT.v[rows, p, jk * 128:(jk + 1) * 128],
                                         rhs=qT.v[rows, p, cs], start=True, stop=False, tile_position=(hh * 64, 0))
                                if not first:
                                    e.matmul(Lp[:, hh, cv:512], lhsT=nonesb, rhs=Rold.v[:, hh, cv:512], start=False,
                                             stop=False)
                                if jj >= 0:
                                    e.matmul(Lp[:, hh, c0:c0 + 128], lhsT=identb, rhs=negsb, start=False, stop=False)
                                ins = e.matmul(Lp[:, hh, cs], lhsT=usufb, rhs=sp_.v[:, hh, cs], start=False, stop=True)
                            return ins
                        fw.op(pe, fL, [KT.b, qT.b, sp_.b, cstb.b] + ([Rold.b] if Rold is not None else []), Lb)
                        AT = ATr.next()
                        fw.op(act, lambda e, Lp=Lp, AT=AT, cs=cs: e.activation(out=AT.v[:, :, cs], in_=Lp[:, :, cs],
                                                                             func=AF.Exp), Lb, [AT.b])
                        if not last:
                            Rnew = Rr.next()
                            if first:
                                fw.op(dve, lambda e, Rnew=Rnew, sp_=sp_, cs=cs: e.tensor_copy(
                                    out=Rnew.v[:, :, cs], in_=sp_.v[:, :, cs]), [sp_.b], [Rnew.b])
                            else:
                                fw.op(dve, lambda e, Rnew=Rnew, Rold=Rold, sp_=sp_, cv=cv: e.tensor_tensor(
                                    out=Rnew.v[:, :, cv:512], in0=Rold.v[:, :, cv:512], in1=sp_.v[:, :, cv:512],
                                    op=ALU.add), [sp_.b, Rold.b], [Rnew.b])
                                if jj >= 0:
                                    fw.op(dve, lambda e, Rnew=Rnew, sp_=sp_, c0=c0, cv=cv: e.tensor_copy(
                                        out=Rnew.v[:, :, c0:cv], in_=sp_.v[:, :, c0:cv]), [sp_.b], [Rnew.b])
                            state["R"] = Rnew

                        state["O%d" % jk] = (AT, cs, first, last)

                    def stageC(jk, p=p, state=state, bkO=bkO):
                        AT, cs, first, last = state.pop("O%d" % jk)

                        def fO(e, AT=AT, cs=cs, jk=jk, first=first, last=last, bkO=bkO):
                            ins = None
                            for hh in range(2):
                                h = 2 * p + hh
                                orow = slice(hh * 64, hh * 64 + 64)
                                tp = (0, hh * 64)
                                if first:
                                    e.matmul(bkO.v[orow, :], lhsT=zerosb[:, 0:64], rhs=qT.v[:, p, :], start=True,
                                             stop=False, tile_position=tp)
                                ins = e.matmul(bkO.v[orow, cs], lhsT=V.v[:, jk, h * 64:(h + 1) * 64],
                                               rhs=AT.v[:, hh, cs], start=False, stop=last, tile_position=tp)
                            return ins
                        fw.op(pe, fO, [AT.b, V.b, qT.b, cstb.b], [bkO.b])

                    def fin(p=p, bkO=bkO):
                        evac(oT.v[:, p, :], bkO.v[:, :], [bkO.b], [oT.b], force="dve")
                    for jk in range(jmax, -1, -1):
                        seq.append((stageA, stageB, stageC, jk, jk == 0, fin))
                n = len(seq)
                for i in range(n + SB_LA + 1):
                    if i < n:
                        seq[i][0](seq[i][3])
                    if SB_LA <= i < n + SB_LA:
                        it = seq[i - SB_LA]
                        it[1](it[3])
                    if i >= SB_LA + 1:
                        it = seq[i - SB_LA - 1]
                        it[2](it[3])
                        if it[4]:
                            it[5]()
                for k in range(4):
                    out_proj(oT, slice(k * 128, (k + 1) * 128), wout, 8, 4 * g + k)

        arena_peak = [0]
        for L in layer_ids:
            if "mix" in PHASES:
                if L % 2 == 0:
                    mlstm_phase(L, L // 2)
                else:
                    sb_phase(L, L // 2)
            if "xa" in PHASES:
                xattn_phase(L)
            if "ffn" in PHASES:
                ffn_phase(L)

        fw.dma(sp, out_d.rearrange("(t p) f -> p t f", p=128), X[:, :, :], XB, [])
        for s_ in fw.dma_sems:
            if s_[2] is not None:
                sp.wait(s_[2])
        fw.emit()
    return nc


def _consts():
    c = np.zeros((10, 128, 128), np.float32)
    i = np.arange(128)
    sp_, t = np.meshgrid(i, i, indexing="ij")
    c[0] = np.eye(128)
    c[1] = (sp_ <= t)
    c[2] = np.where(sp_ > t, -30000.0, 0.0)
    c[3] = np.where(sp_ >= t, -1.0, 0.0)
    c[4] = -1.0
    c[5] = np.where(sp_ >= t, -30000.0, 0.0)
    c[6] = (sp_ < t)
    c[7] = 1.0
    c[8] = 0.0
    cf = np.ascontiguousarray(c[[0, 1, 2, 7]].transpose(1, 0, 2))
    cb = np.ascontiguousarray(c[[0, 3, 4, 5, 6, 8]].transpose(1, 0, 2))
    return cf, cb


def _prep_shared(mem_norm_gain, norm_gains, ml_b_gate, ml_head_gain):
    g_all = np.concatenate([norm_gains.reshape(24, D), mem_norm_gain.reshape(1, D)], 0)
    gcol = np.ascontiguousarray(g_all.reshape(25, 8, 128).transpose(2, 0, 1).reshape(128, 200)).astype(np.float32)
    post = norm_gains[:, [1, 3, 5], :].reshape(12, 1, D)
    gbc = np.ascontiguousarray(np.broadcast_to(post, (12, 128, D))).astype(np.float32)
    hgbc = np.ascontiguousarray(np.broadcast_to(ml_head_gain.reshape(2, 1, D), (2, 128, D))).astype(np.float32)
    bgate = np.ascontiguousarray(np.broadcast_to(ml_b_gate.reshape(2, 1, 8), (2, 128, 8))).astype(np.float32)
    return gcol, gbc, hgbc, bgate


_PROG_CACHE = {}


def _get_prog(layer_ids):
    key = tuple(layer_ids)
    if key not in _PROG_CACHE:
        _PROG_CACHE[key] = build_program(list(layer_ids))
    return _PROG_CACHE[key]


LAUNCH_GROUPS = [[0, 1, 2, 3]]


def kernel(x, mem, mem_norm_gain, norm_gains, ml_w_in, ml_b_gate, ml_head_gain, ml_w_out,
           sb_w_qkv, sb_w_out, xa_w_q, xa_w_kv, xa_w_o, ffn_w_gate_up, ffn_w_down):
    f = lambda a: np.ascontiguousarray(np.asarray(a, dtype=np.float32))
    x = f(x)
    mem = f(mem)
    gcol, gbc, hgbc, bgate = _prep_shared(f(mem_norm_gain), f(norm_gains), f(ml_b_gate), f(ml_head_gain))
    shared = {
        "gcol": gcol, "gbc": gbc, "hgbc": hgbc, "bgate": bgate, "constf": _consts()[0], "constb": _consts()[1],
        "ml_w_in": f(ml_w_in), "ml_w_out": f(ml_w_out), "sb_w_qkv": f(sb_w_qkv), "sb_w_out": f(sb_w_out),
        "xa_w_q": f(xa_w_q), "xa_w_kv": f(xa_w_kv), "xa_w_o": f(xa_w_o),
        "ffn_w_gate_up": f(ffn_w_gate_up), "ffn_w_down": f(ffn_w_down),
    }
    cur = [x[b] for b in range(NCORES)]
    for grp in LAUNCH_GROUPS:
        nc = _get_prog(grp)
        in_maps = []
        for b in range(NCORES):
            m = dict(shared)
            m["x"] = cur[b]
            m["mem"] = mem[b]
            in_maps.append(m)
        res = run_bass_kernel_spmd(nc, in_maps, core_ids=list(range(NCORES)))
        cur = [np.asarray(res.results[b]["out"], dtype=np.float32) for b in range(NCORES)]
    return np.stack(cur, 0)
```
